# Optimizing a Trainium2 kernel written in Bass

```python
import math
import jax
import jax.numpy as jnp
from jax import lax
import numpy as np

D_MODEL = 1024
BATCH = 2
SEQ = 8192
DEPTH = 4

GRID_W = 64
CTX_LEN = 256
ROPE_THETA = 10000.0
NORM_EPS = 1e-6
Q_BLOCK = 128

A_HEADS = 4
A_HD = 64
A_VD = 2 * A_HD
A_SCALE = A_HD ** -0.5
A_PROJ = 4 * A_HEADS * A_HD + A_HEADS * A_VD

B_HEADS = 4
B_NOPE = 128
B_ROPE = 64
B_VD = 128
B_Q_RANK = 256
B_KV_RANK = 128
B_SCALE = (B_NOPE + B_ROPE) ** -0.5
B_PROJ = B_Q_RANK + B_KV_RANK + B_ROPE

AB_PROJ = A_PROJ + B_PROJ
AB_OUT = A_HEADS * A_VD + B_HEADS * B_VD

C_HEADS = 8
C_HD = D_MODEL // C_HEADS
C_WIDTH = C_HEADS * C_HD
C_PROJ = 5 * C_WIDTH
C_CHUNK = 64

N_EXPERTS = 32
TOP_K = 4
D_FF = D_MODEL
SWIGLU_ALPHA = 1.702
SWIGLU_LIMIT = 7.0
MOE_BLOCK = 128

N_AB_LAYERS = (DEPTH + 1) // 2
N_C_LAYERS = DEPTH // 2

kernel_name = 'hybrid_diff_mla_hgrn2_moe'


def rms_norm(x, g):
    xf = x.astype(jnp.float32)
    y = xf * lax.rsqrt(jnp.mean(xf * xf, axis=-1, keepdims=True) + NORM_EPS)
    return (y * g.astype(jnp.float32)).astype(x.dtype)


def axial_rope_tables(rows, rot_dim):
    quarter = rot_dim // 4
    inv_freq = ROPE_THETA ** (-jnp.arange(quarter, dtype=jnp.float32) / quarter)
    t = jnp.arange(rows * GRID_W)
    row = (t // GRID_W).astype(jnp.float32)
    col = (t % GRID_W).astype(jnp.float32)
    ang_r = row[:, None] * inv_freq
    ang_c = col[:, None] * inv_freq
    return (jnp.cos(ang_r), jnp.sin(ang_r), jnp.cos(ang_c), jnp.sin(ang_c))


def _rotate(x, cos, sin):
    x1, x2 = jnp.split(x, 2, axis=-1)
    return jnp.concatenate([x1 * cos - x2 * sin, x2 * cos + x1 * sin], axis=-1)


def apply_axial_rope(x, tables):
    cr, sr, cc, sc = tables
    xr, xc = jnp.split(x, 2, axis=-1)
    return jnp.concatenate([_rotate(xr, cr, sr), _rotate(xc, cc, sc)], axis=-1).astype(x.dtype)


def merge_heads(o):
    b, h, t, d = o.shape
    return o.transpose(0, 2, 1, 3).reshape(b, t, h * d)


def context_attention(q, k, v, scale):
    s = jnp.einsum('bhqd,bhkd->bhqk', q, k, preferred_element_type=jnp.float32) * scale
    p = jax.nn.softmax(s, axis=-1)
    return jnp.einsum('bhqk,bhkd->bhqd', p.astype(v.dtype), v)


def latent_attention(q, k_lat, v_lat, k_ctx, v_ctx, scale):
    k = jnp.concatenate([k_ctx, k_lat], axis=2)
    v = jnp.concatenate([v_ctx, v_lat], axis=2)
    b, h, n, dk = q.shape
    nb = n // Q_BLOCK
    qb = jnp.moveaxis(q.reshape(b, h, nb, Q_BLOCK, dk), 2, 0)
    ob = lax.map(lambda qi: context_attention(qi, k, v, scale), qb)
    return jnp.moveaxis(ob, 0, 2).reshape(b, h, n, v.shape[-1])


def diff_heads(p, rope):
    b, t, _ = p.shape
    q, k, v = jnp.split(p, [2 * A_HEADS * A_HD, 4 * A_HEADS * A_HD], axis=-1)
    q = q.reshape(b, t, 2 * A_HEADS, A_HD).transpose(0, 2, 1, 3)
    k = k.reshape(b, t, 2 * A_HEADS, A_HD).transpose(0, 2, 1, 3)
    v = v.reshape(b, t, A_HEADS, A_VD).transpose(0, 2, 1, 3)
    v = jnp.repeat(v, 2, axis=1)
    if rope is not None:
        q = apply_axial_rope(q, rope)
        k = apply_axial_rope(k, rope)
    return q, k, v


def diff_combine(o2, lam, lam_init, subln_g):
    b, _, t, vd = o2.shape
    o2 = o2.reshape(b, A_HEADS, 2, t, vd)
    o = o2[:, :, 0] - lam * o2[:, :, 1]
    o = rms_norm(o, subln_g) * (1.0 - lam_init)
    return merge_heads(o)


def mla_heads(p, q_norm_g, kv_norm_g, w_uq, w_ukv, rope):
    b, t, _ = p.shape
    c_q, c_kv, k_r = jnp.split(p, [B_Q_RANK, B_Q_RANK + B_KV_RANK], axis=-1)
    q = (rms_norm(c_q, q_norm_g) @ w_uq).reshape(b, t, B_HEADS, B_NOPE + B_ROPE).transpose(0, 2, 1, 3)
    kv = (rms_norm(c_kv, kv_norm_g) @ w_ukv).reshape(b, t, B_HEADS, B_NOPE + B_VD).transpose(0, 2, 1, 3)
    q_nope, q_r = jnp.split(q, [B_NOPE], axis=-1)
    k_nope, v = jnp.split(kv, [B_NOPE], axis=-1)
    k_r = k_r[:, None]
    if rope is not None:
        q_r = apply_axial_rope(q_r, rope)
        k_r = apply_axial_rope(k_r, rope)
    q = jnp.concatenate([q_nope, q_r], axis=-1)
    k = jnp.concatenate([k_nope, jnp.broadcast_to(k_r, (b, B_HEADS, t, B_ROPE))], axis=-1)
    return q, k, v


def ab_mixer(u_lat, u_ctx, w_in, diff_lambda, subln_g, q_norm_g, kv_norm_g, w_uq, w_ukv, w_out,
             lam_init, rope_a, rope_b, with_ctx_out):
    p_lat = u_lat @ w_in
    p_ctx = u_ctx @ w_in
    lf = diff_lambda.astype(jnp.float32)
    lam = jnp.exp(jnp.sum(lf[0] * lf[1])) - jnp.exp(jnp.sum(lf[2] * lf[3])) + lam_init
    qa, ka, va = diff_heads(p_lat[..., :A_PROJ], rope_a)
    qac, kac, vac = diff_heads(p_ctx[..., :A_PROJ], None)
    qb, kb, vb = mla_heads(p_lat[..., A_PROJ:], q_norm_g, kv_norm_g, w_uq, w_ukv, rope_b)
    qbc, kbc, vbc = mla_heads(p_ctx[..., A_PROJ:], q_norm_g, kv_norm_g, w_uq, w_ukv, None)
    oa = diff_combine(latent_attention(qa, ka, va, kac, vac, A_SCALE), lam, lam_init, subln_g)
    ob = merge_heads(latent_attention(qb, kb, vb, kbc, vbc, B_SCALE))
    y_lat = jnp.concatenate([oa, ob], axis=-1).astype(u_lat.dtype) @ w_out
    y_ctx = None
    if with_ctx_out:
        oac = diff_combine(context_attention(qac, kac, vac, A_SCALE), lam, lam_init, subln_g)
        obc = merge_heads(context_attention(qbc, kbc, vbc, B_SCALE))
        y_ctx = jnp.concatenate([oac, obc], axis=-1).astype(u_ctx.dtype) @ w_out
    return y_lat, y_ctx


def gla_chunk_scan(q, k, v, log_f, s0):
    b, h, t, dk = q.shape
    dv = v.shape[-1]
    n = t // C_CHUNK

    def to_chunks(a):
        return jnp.moveaxis(a.reshape(b, h, n, C_CHUNK, a.shape[-1]), 2, 0)

    mask = jnp.tril(jnp.ones((C_CHUNK, C_CHUNK), dtype=bool))[:, :, None]

    def step(s, inp):
        qc, kc, vc, lf = inp
        cum = jnp.cumsum(lf, axis=2)
        rel = jnp.where(mask, cum[:, :, :, None, :] - cum[:, :, None, :, :], -jnp.inf)
        att = jnp.einsum('bhtd,bhsd,bhtsd->bhts', qc, kc, jnp.exp(rel))
        o = jnp.einsum('bhts,bhse->bhte', att, vc) + jnp.einsum('bhtd,bhde->bhte', qc * jnp.exp(cum), s)
        last = cum[:, :, -1]
        s_new = jnp.exp(last)[..., None] * s + jnp.einsum(
            'bhsd,bhse->bhde', kc * jnp.exp(last[:, :, None, :] - cum), vc)
        return s_new, o

    s_fin, o = lax.scan(step, s0, (to_chunks(q), to_chunks(k), to_chunks(v), to_chunks(log_f)))
    return jnp.moveaxis(o, 0, 2).reshape(b, h, t, dv), s_fin


def hgrn2_mixer(u_lat, u_ctx, w_in, lb, norm_g, w_out, with_ctx_out):
    lbh = lb.astype(jnp.float32).reshape(C_HEADS, 1, C_HD)

    def project(u):
        b, t, _ = u.shape
        q, zf, zb, i, g = jnp.split(u @ w_in, 5, axis=-1)

        def heads(a):
            return a.reshape(b, t, C_HEADS, C_HD).transpose(0, 2, 1, 3)

        def gate(z):
            f = lbh + (1.0 - lbh) * jax.nn.sigmoid(heads(z).astype(jnp.float32))
            return 1.0 - f, jnp.log(f)

        return heads(q), heads(i), gate(zf), gate(zb), g

    def readout(o, g):
        b, _, t, _ = o.shape
        o = rms_norm(o, norm_g).transpose(0, 2, 1, 3).reshape(b, t, C_WIDTH)
        return (o * jax.nn.silu(g.astype(jnp.float32))).astype(g.dtype) @ w_out

    def flip(a):
        return jnp.flip(a, axis=2)

    q_c, v_c, (kf_c, lf_c), (kb_c, lgb_c), g_c = project(u_ctx)
    s0 = jnp.zeros((u_ctx.shape[0], C_HEADS, C_HD, C_HD), jnp.float32)
    o_cf, s_f = gla_chunk_scan(q_c, kf_c, v_c, lf_c, s0)
    o_cb, s_b = gla_chunk_scan(flip(q_c), flip(kb_c), flip(v_c), flip(lgb_c), s0)
    q_l, v_l, (kf_l, lf_l), (kb_l, lgb_l), g_l = project(u_lat)
    o_lf, _ = gla_chunk_scan(q_l, kf_l, v_l, lf_l, s_f)
    o_lb, _ = gla_chunk_scan(flip(q_l), flip(kb_l), flip(v_l), flip(lgb_l), s_b)
    y_lat = readout(o_lf + flip(o_lb), g_l)
    y_ctx = readout(o_cf + flip(o_cb), g_c) if with_ctx_out else None
    return y_lat, y_ctx


def expert_ffn(xb, w1, b1, w2, b2):
    h = jnp.dot(xb, w1) + b1
    x_glu = jnp.minimum(h[:, 0::2], SWIGLU_LIMIT)
    x_lin = jnp.clip(h[:, 1::2], -SWIGLU_LIMIT, SWIGLU_LIMIT)
    y = x_glu * jax.nn.sigmoid(SWIGLU_ALPHA * x_glu) * (x_lin + 1.0)
    return jnp.dot(y, w2) + b2


def moe_ffn(tok, w_router, b_router, w1, b1, w2, b2):
    t, d = tok.shape
    logits = jnp.einsum('td,de->te', tok, w_router, preferred_element_type=jnp.float32) + b_router.astype(jnp.float32)
    top_val, top_idx = lax.top_k(logits, TOP_K)
    gates = jax.nn.softmax(top_val, axis=-1)
    n_assign = t * TOP_K
    flat_e = top_idx.reshape(-1).astype(jnp.int32)
    flat_tok = jnp.arange(n_assign, dtype=jnp.int32) // TOP_K
    flat_g = gates.reshape(-1)
    order = jnp.argsort(flat_e)
    e_sorted = flat_e[order]
    counts = jnp.bincount(flat_e, length=N_EXPERTS).astype(jnp.int32)
    group_start = jnp.cumsum(counts) - counts
    padded = (counts + MOE_BLOCK - 1) // MOE_BLOCK * MOE_BLOCK
    pad_end = jnp.cumsum(padded)
    pad_start = pad_end - padded
    dest = pad_start[e_sorted] + (jnp.arange(n_assign, dtype=jnp.int32) - group_start[e_sorted])
    n_blocks = -(-n_assign // MOE_BLOCK) + N_EXPERTS
    n_rows = n_blocks * MOE_BLOCK
    row_tok = jnp.full((n_rows,), t, jnp.int32).at[dest].set(flat_tok[order])
    row_gate = jnp.zeros((n_rows,), jnp.float32).at[dest].set(flat_g[order])
    block_start = jnp.arange(n_blocks, dtype=jnp.int32) * MOE_BLOCK
    block_expert = jnp.minimum(jnp.searchsorted(pad_end, block_start, side='right'), N_EXPERTS - 1)
    tok_pad = jnp.concatenate([tok, jnp.zeros((1, d), tok.dtype)], axis=0)
    xb = tok_pad[row_tok].reshape(n_blocks, MOE_BLOCK, d)
    yb = lax.map(lambda a: expert_ffn(a[0], w1[a[1]], b1[a[1]], w2[a[1]], b2[a[1]]), (xb, block_expert))
    y = yb.reshape(n_rows, d) * row_gate[:, None].astype(yb.dtype)
    return jnp.zeros((t + 1, d), y.dtype).at[row_tok].add(y)[:t]


def setup_inputs(seed: int = 0) -> dict:
    key = jax.random.key(seed)
    keys = iter(jax.random.split(key, 40))

    def nrm(shape, scale):
        return jax.random.normal(next(keys), shape, jnp.float32) * scale

    def gain(shape):
        return 1.0 + nrm(shape, 0.05)

    D = D_MODEL
    return {
        'x': nrm((BATCH, SEQ, D), 1.0),
        'c': nrm((BATCH, D), 1.0),
        'ctx': nrm((BATCH, CTX_LEN, D), 1.0),
        'c_ctx': nrm((D,), 1.0),
        'norm_mix_g': gain((DEPTH, D)),
        'norm_ffn_g': gain((DEPTH, D)),
        'w_ada': nrm((DEPTH, D, 6 * D), 0.3 * D ** -0.5),
        'b_ada': nrm((DEPTH, 6 * D), 0.02),
        'w_in_ab': nrm((N_AB_LAYERS, D, AB_PROJ), D ** -0.5),
        'diff_lambda': nrm((N_AB_LAYERS, 4, A_HD), 0.1),
        'diff_subln_g': gain((N_AB_LAYERS, A_VD)),
        'mla_q_norm_g': gain((N_AB_LAYERS, B_Q_RANK)),
        'mla_kv_norm_g': gain((N_AB_LAYERS, B_KV_RANK)),
        'w_uq': nrm((N_AB_LAYERS, B_Q_RANK, B_HEADS * (B_NOPE + B_ROPE)), B_Q_RANK ** -0.5),
        'w_ukv': nrm((N_AB_LAYERS, B_KV_RANK, B_HEADS * (B_NOPE + B_VD)), B_KV_RANK ** -0.5),
        'w_out_ab': nrm((N_AB_LAYERS, AB_OUT, D), AB_OUT ** -0.5),
        'w_in_c': nrm((N_C_LAYERS, D, C_PROJ), D ** -0.5),
        'lb_raw': nrm((DEPTH, C_WIDTH), 0.1),
        'hgrn_norm_g': gain((N_C_LAYERS, C_HD)),
        'w_out_c': nrm((N_C_LAYERS, C_WIDTH, D), C_WIDTH ** -0.5),
        'w_router': nrm((DEPTH, D, N_EXPERTS), D ** -0.5),
        'b_router': nrm((DEPTH, N_EXPERTS), 0.01),
        'w_exp1': nrm((DEPTH, N_EXPERTS, D, 2 * D_FF), D ** -0.5),
        'b_exp1': nrm((DEPTH, N_EXPERTS, 2 * D_FF), 0.01),
        'w_exp2': nrm((DEPTH, N_EXPERTS, D_FF, D), D_FF ** -0.5),
        'b_exp2': nrm((DEPTH, N_EXPERTS, D), 0.01),
        'final_g': gain((D,)),
    }


def reference(x, c, ctx, c_ctx, norm_mix_g, norm_ffn_g, w_ada, b_ada, w_in_ab, diff_lambda, diff_subln_g,
              mla_q_norm_g, mla_kv_norm_g, w_uq, w_ukv, w_out_ab, w_in_c, lb_raw, hgrn_norm_g, w_out_c,
              w_router, b_router, w_exp1, b_exp1, w_exp2, b_exp2, final_g):
    b, n, d = x.shape
    rows = n // GRID_W
    rope_a = axial_rope_tables(rows, A_HD)
    rope_b = axial_rope_tables(rows, B_ROPE)
    lb_p = jax.nn.softmax(lb_raw.astype(jnp.float32), axis=0)
    lower_bounds = jnp.cumsum(lb_p, axis=0) - lb_p[0]
    silu_c = jax.nn.silu(c)
    silu_cc = jax.nn.silu(c_ctx)
    h, hc = x, ctx
    for l in range(DEPTH):
        last = l == DEPTH - 1
        sh1, sc1, g1, sh2, sc2, g2 = jnp.split((silu_c @ w_ada[l] + b_ada[l])[:, None, :], 6, axis=-1)
        csh1, csc1, cg1, csh2, csc2, cg2 = jnp.split(silu_cc @ w_ada[l] + b_ada[l], 6, axis=-1)
        u = rms_norm(h, norm_mix_g[l]) * (1.0 + sc1) + sh1
        uc = rms_norm(hc, norm_mix_g[l]) * (1.0 + csc1) + csh1
        j = l // 2
        if l % 2 == 0:
            lam_init = 0.8 - 0.6 * math.exp(-0.3 * l)
            y, yc = ab_mixer(u, uc, w_in_ab[j], diff_lambda[j], diff_subln_g[j], mla_q_norm_g[j],
                             mla_kv_norm_g[j], w_uq[j], w_ukv[j], w_out_ab[j], lam_init, rope_a, rope_b,
                             not last)
        else:
            y, yc = hgrn2_mixer(u, uc, w_in_c[j], lower_bounds[l], hgrn_norm_g[j], w_out_c[j], not last)
        h = h + g1 * y
        v = rms_norm(h, norm_ffn_g[l]) * (1.0 + sc2) + sh2
        moe_w = (w_router[l], b_router[l], w_exp1[l], b_exp1[l], w_exp2[l], b_exp2[l])
        if last:
            h = h + g2 * moe_ffn(v.reshape(b * n, d), *moe_w).reshape(b, n, d)
        else:
            hc = hc + cg1 * yc
            vc = rms_norm(hc, norm_ffn_g[l]) * (1.0 + csc2) + csh2
            out = moe_ffn(jnp.concatenate([v.reshape(b * n, d), vc.reshape(-1, d)], axis=0), *moe_w)
            h = h + g2 * out[:b * n].reshape(b, n, d)
            hc = hc + cg2 * out[b * n:].reshape(hc.shape)
    return rms_norm(h, final_g)
```

```python
import math
from contextlib import ExitStack
import numpy as np
import concourse.bass as bass
import concourse.mybir as mybir
from concourse.bass_utils import run_bass_kernel_spmd

F32 = mybir.dt.float32
BF16 = mybir.dt.bfloat16
I32 = mybir.dt.int32
AF = mybir.ActivationFunctionType
ALU = mybir.AluOpType
AX = mybir.AxisListType

D = 1024
NCH = 8
NLAT = 2048
NCTX = 64
T = NLAT + NCTX
SEQ = 8192
CTX = 256
NKEY = SEQ + CTX
DEPTH = 4
NE = 32
EPS = 1e-6
TILES = [(0, 512), (512, 512), (1024, 512), (1536, 512), (2048, 64)]

EPOCH = 30000


class Tr:
    __slots__ = ("name", "w", "rs", "rd", "ps")

    def __init__(self, name="", ps=False):
        self.name = name
        self.ps = ps
        self.w = None
        self.rs = {}
        self.rd = []


class Op:
    __slots__ = ("eng", "fn", "dma", "deps", "ddeps", "signal", "sidx", "dsem", "dval", "idx")

    def __init__(self, eng, fn, dma):
        self.eng = eng
        self.fn = fn
        self.dma = dma
        self.deps = {}
        self.ddeps = []
        self.signal = False
        self.sidx = 0
        self.dsem = None
        self.dval = 0
        self.idx = 0


ENGS = ("pe", "dve", "act", "pool", "sp")
NDMASEM = {"sp": 12, "pool": 8, "act": 4, "dve": 2, "pe": 2}


class Prog:
    def __init__(self, nc):
        self.nc = nc
        self.stack = ExitStack()
        self.left = self.SB_LO
        self.left_persist = self.SB_LO
        self.right = self.SB_HI
        self.nbank = 0
        self.banks = [self.stack.enter_context(nc.psum_tensor(f"bank{i}", [128, 512], F32)) for i in range(8)]
        self.nbuf = 0
        self.sems = {e: [] for e in ENGS}
        self.dsems = {e: [] for e in ENGS}
        self.sbase = {e: 0 for e in ENGS}
        self.dcount = {e: 0 for e in ENGS}
        self.clock = {e: {} for e in ENGS}
        self.gidx = {e: 0 for e in ENGS}
        self._reset()

    def _reset(self):
        self.ops = {e: [] for e in ENGS}
        self.dmaq = {e: [] for e in ENGS}
        self.all_dma = []

    SB_LO = 16512
    SB_HI = 229344

    def sbuf(self, name, shape, dtype, persist=False, mid=False):
        self.nbuf += 1
        n = 1
        for d_ in shape[1:]:
            n *= d_
        nbytes = n * (2 if dtype == BF16 else 4)
        nbytes = (nbytes + 63) // 64 * 64
        if persist or mid:
            off = self.left
            self.left += nbytes
        else:
            self.right -= nbytes
            off = self.right
        assert self.left <= self.right, f"SBUF overflow allocating {name}: left={self.left} right={self.right}"
        return self.nc.alloc_sbuf_tensor_at(f"{name}_{self.nbuf}", list(shape), dtype, offset=off)

    def psum(self, name=None, shape=None, dtype=None):
        assert self.nbank < 8, "out of PSUM banks"
        t = self.banks[self.nbank]
        self.nbank += 1
        return t

    def _need(self, o, p, raw):
        if p is None or p is o:
            return
        if p.dma:
            if p not in o.ddeps:
                o.ddeps.append(p)
            return
        if (not o.dma) and p.eng == o.eng:
            if not raw or o.eng == "pe":
                return
        cur = o.deps.get(p.eng)
        if cur is None or cur.idx < p.idx:
            o.deps[p.eng] = p
        p.signal = True

    def op(self, eng, fn, reads=(), writes=(), dma=False):
        o = Op(eng, fn, dma)
        self.gidx[eng] += 1
        o.idx = self.gidx[eng]
        for t in reads:
            self._need(o, t.w, True)
            if t.ps:
                for r in t.rs.values():
                    if r.eng != eng:
                        self._need(o, r, False)
        for t in writes:
            self._need(o, t.w, False)
            for r in t.rs.values():
                self._need(o, r, False)
            for r in t.rd:
                self._need(o, r, False)
        if dma:
            q = self.dmaq[eng]
            n = NDMASEM[eng]
            if len(q) >= n:
                self._need(o, q[len(q) - n], False)
            q.append(o)
            self.all_dma.append(o)
        for t in reads:
            if dma:
                t.rd.append(o)
            else:
                t.rs[eng] = o
        for t in writes:
            t.w = o
            t.rs = {}
            t.rd = []
        self.ops[eng].append(o)
        return o

    def flush(self):
        nc = self.nc
        lasts = []
        for e in ENGS:
            for o in reversed(self.ops[e]):
                if not o.dma:
                    lasts.append(o)
                    break
        for e in ENGS:
            o = Op(e, None, False)
            self.gidx[e] += 1
            o.idx = self.gidx[e]
            for p in lasts:
                if p.eng != e:
                    cur = o.deps.get(p.eng)
                    o.deps[p.eng] = p
                    p.signal = True
            o.ddeps = list(self.all_dma)
            self.ops[e].append(o)
        for e in ENGS:
            c = self.sbase[e]
            for o in self.ops[e]:
                if o.signal and not o.dma:
                    c += 1
                    o.sidx = c
            self.sbase[e] = c
            need = (c + EPOCH - 1) // EPOCH
            while len(self.sems[e]) < need:
                self.sems[e].append(self.stack.enter_context(nc.semaphore(f"s_{e}_{len(self.sems[e])}")))
        for e in ENGS:
            n = NDMASEM[e]
            if self.dmaq[e]:
                while len(self.dsems[e]) < n:
                    self.dsems[e].append(self.stack.enter_context(nc.semaphore(f"d_{e}_{len(self.dsems[e])}")))
            for o in self.dmaq[e]:
                i = self.dcount[e]
                self.dcount[e] += 1
                o.dsem = (e, i % n)
                o.dval = 16 * (i // n + 1)
        sems, dsems = self.sems, self.dsems
        self._check_deadlock()

        def run_engine(e, eng):
            clock = self.clock[e]
            for o in self.ops[e]:
                for f, p in o.deps.items():
                    if clock.get(f, 0) < p.sidx:
                        s = p.sidx - 1
                        eng.wait_ge(sems[f][s // EPOCH], s % EPOCH + 1)
                        clock[f] = p.sidx
                for p in o.ddeps:
                    key = ("d",) + p.dsem
                    if clock.get(key, 0) < p.dval:
                        eng.wait_ge(dsems[p.dsem[0]][p.dsem[1]], p.dval)
                        clock[key] = p.dval
                if o.dma:
                    key = ("d",) + o.dsem
                    if clock.get(key, 0) < o.dval - 16:
                        eng.wait_ge(dsems[o.dsem[0]][o.dsem[1]], o.dval - 16)
                        clock[key] = o.dval - 16
                if o.fn is None:
                    continue
                ins = o.fn(eng)
                if o.dma:
                    ins.then_inc(dsems[o.dsem[0]][o.dsem[1]], 16)
                elif o.signal:
                    s = o.sidx - 1
                    ins.then_inc(sems[e][s // EPOCH], 1)

        with nc.Block() as block:
            @block.sync
            def _(eng):
                run_engine("sp", eng)

            @block.tensor
            def _(eng):
                run_engine("pe", eng)

            @block.vector
            def _(eng):
                run_engine("dve", eng)

            @block.scalar
            def _(eng):
                run_engine("act", eng)

            @block.gpsimd
            def _(eng):
                run_engine("pool", eng)
        self._reset()
        self.right = self.SB_HI
        self.nbank = 0

    def _check_deadlock(self):
        cnt = dict(self._sim_cnt) if hasattr(self, "_sim_cnt") else {}
        pos = {e: 0 for e in ENGS}
        progress = True
        while progress:
            progress = False
            for e in ENGS:
                ops = self.ops[e]
                while pos[e] < len(ops):
                    o = ops[pos[e]]
                    ok = True
                    for f, p in o.deps.items():
                        if p.sidx and cnt.get(("s", f), 0) < p.sidx:
                            ok = False
                    for p in o.ddeps:
                        if cnt.get(("d",) + p.dsem, 0) < p.dval:
                            ok = False
                    if o.dma and cnt.get(("d",) + o.dsem, 0) < o.dval - 16:
                        ok = False
                    if not ok:
                        break
                    if o.dma:
                        cnt[("d",) + o.dsem] = cnt.get(("d",) + o.dsem, 0) + 16
                    elif o.signal:
                        assert cnt.get(("s", e), 0) == o.sidx - 1, (e, cnt.get(("s", e), 0), o.sidx)
                        cnt[("s", e)] = o.sidx
                    pos[e] += 1
                    progress = True
        for e in ENGS:
            assert pos[e] == len(self.ops[e]), f"DEADLOCK: engine {e} stuck at op {pos[e]}/{len(self.ops[e])}"
        self._sim_cnt = cnt

    def mark(self):
        return self.left

    def release(self, m):
        self.left = m

    def close(self):
        self.stack.close()

    def mm(self, out, lhsT, rhs, start, stop, reads, writes):
        return self.op("pe", lambda e: e.matmul(out, lhsT, rhs, start=start, stop=stop), reads, writes)

    def tp(self, out, in_, ident, reads, writes):
        return self.op("pe", lambda e: e.transpose(out, in_, ident), reads, writes)

    def dma(self, eng, out, in_, reads, writes):
        return self.op(eng, lambda e: e.dma_start(out=out, in_=in_), reads, writes, dma=True)

    def act(self, out, in_, func, reads, writes, bias=None, scale=None):
        kw = {}
        if bias is not None:
            kw["bias"] = bias
        if scale is not None:
            kw["scale"] = scale
        return self.op("act", lambda e: e.activation(out, in_, func, **kw), reads, writes)

    def ts(self, eng, out, in0, s1, s2, op0, op1, reads, writes):
        if op1 is None:
            return self.op(eng, lambda e: e.tensor_scalar(out, in0, s1, None, op0), reads, writes)
        return self.op(eng, lambda e: e.tensor_scalar(out, in0, s1, s2, op0, op1), reads, writes)

    def tt(self, eng, out, in0, in1, op, reads, writes):
        return self.op(eng, lambda e: e.tensor_tensor(out, in0, in1, op), reads, writes)

    def stt(self, eng, out, in0, scalar, in1, op0, op1, reads, writes):
        return self.op(eng, lambda e: e.scalar_tensor_tensor(out, in0, scalar, in1, op0, op1), reads, writes)

    def copy(self, eng, out, in_, reads, writes):
        if eng == "act":
            return self.op("act", lambda e: e.copy(out, in_), reads, writes)
        return self.op(eng, lambda e: e.tensor_copy(out, in_), reads, writes)

    def memset(self, eng, ap, val, writes):
        return self.op(eng, lambda e: e.memset(ap, val), (), writes)


class Rot:
    def __init__(self, items):
        self.items = items
        self.i = 0

    def next(self):
        it = self.items[self.i % len(self.items)]
        self.i += 1
        return it


class LB:
    def __init__(self):
        self.nc = bass.Bass("TRN2", target_bir_lowering=False)
        self.P = Prog(self.nc)
        self.ins = {}
        self.outs = {}

    def inp(self, name, shape, dtype=F32):
        t = self.nc.dram_tensor(name, list(shape), dtype, kind="ExternalInput").ap()
        self.ins[name] = t
        return t

    def out(self, name, shape, dtype=F32):
        t = self.nc.dram_tensor(name, list(shape), dtype, kind="ExternalOutput").ap()
        self.outs[name] = t
        return t

    def consts(self):
        P = self.P
        ident_d = self.inp("ident", [128, 128])
        self.identf = P.sbuf("identf", [128, 128], F32, persist=True)
        self.identf_t = Tr()
        self.identb = P.sbuf("identb", [128, 128], BF16, persist=True)
        self.identb_t = Tr()
        self.onesb = P.sbuf("onesb", [128, 128], BF16, persist=True)
        self.onesb_t = Tr()
        P.dma("sp", self.identf[:], ident_d[:, :], [], [self.identf_t])
        P.dma("pool", self.identb[:], ident_d[:, :], [], [self.identb_t])
        P.memset("dve", self.onesb[:], 1.0, [self.onesb_t])
        self.cst = P.sbuf("cst", [128, 4], F32, persist=True)
        self.cst_t = Tr()
        for i_, v_ in enumerate((7.0, -6.0, 8.0, 1.0)):
            P.memset("dve", self.cst[:, i_:i_ + 1], v_, [self.cst_t])
        self.kmaxA = P.sbuf("kmaxA", [128, 1], F32, persist=True)
        self.kmaxB = P.sbuf("kmaxB", [128, 2], F32, persist=True)
        self.kmaxA_t = Tr()
        self.kmaxB_t = Tr()
        cT_d = self.inp("cT", [128, 16])
        self.scT = P.sbuf("scT", [128, 8, 2], F32, persist=True)
        self.scT_t = Tr()
        cT = P.sbuf("cTf", [128, 16], F32)
        cT_t = Tr()
        P.dma("sp", cT[:], cT_d[:, :], [], [cT_t])
        P.act(self.scT[:].rearrange("p c n -> p (c n)"), cT[:], AF.Silu, [cT_t], [self.scT_t])

    def mods(self, l):
        P = self.P
        wada = self.inp(f"wada{l}", [D, 6 * D])
        vec_d = self.inp(f"vec{l}", [128, 64])
        vec = P.sbuf("vec", [128, 64], F32, persist=True)
        vec_t = Tr()
        P.dma("sp", vec[:], vec_d[:, :], [], [vec_t])
        mod = P.sbuf("mod", [128, 48, 2], F32, persist=True)
        mod_t = Tr()
        wv = wada.rearrange("(k p) f -> p k f", p=128)
        wp = [(P.sbuf(f"wadap{i}", [128, 8, 512], F32), Tr()) for i in range(2)]
        ps = [(P.psum(f"modps{i}", [128, 512], F32), Tr(ps=True)) for i in range(2)]
        for i in range(12):
            wt, wt_t = wp[i % 2]
            P.dma("sp", wt[:], wv[:, :, i * 512:(i + 1) * 512], [], [wt_t])
            for fc in range(4):
                q = i * 4 + fc
                pt, pt_t = ps[q % 2]
                for k in range(8):
                    P.mm(pt[:, 0:2], wt[:, k, fc * 128:(fc + 1) * 128], self.scT[:, k, :], k == 0, k == 7,
                         [wt_t, self.scT_t], [pt_t])
                P.act(mod[:, q, :], pt[:, 0:2], AF.Identity, [pt_t, vec_t], [mod_t], bias=vec[:, q:q + 1])
        AB = P.sbuf("modAB", [128, 2, 8, 2], F32, persist=True)
        AB_t = Tr()
        for w_i, (sc0, g0) in enumerate(((8, 48), (32, 56))):
            for col in range(2):
                P.stt("dve", AB[:, w_i, :, col], mod[:, sc0:sc0 + 8, col], 1.0, vec[:, g0:g0 + 8], ALU.add, ALU.mult,
                      [mod_t, vec_t], [AB_t])
        self.mod, self.mod_t, self.AB, self.AB_t = mod, mod_t, AB, AB_t

    def mod_ap(self, which, c, col):
        return self.mod[:, which * 8 + c, col:col + 1]

    def norm_mod(self, hT, hT_t, uT, uT_t, which, f32cb=None, tile_cb=None, hget=None, tile_pre=None):
        P = self.P
        if hget is None:
            hget = lambda ti, c, s, w: hT[:, c, s:s + w]
        sq = [(P.sbuf(f"nm_sq{i}", [128, 8, 512], BF16), Tr()) for i in range(1)]
        ssp = [(P.psum(f"nm_ss{i}", [128, 512], F32), Tr(ps=True)) for i in range(2)]
        rs = [(P.sbuf(f"nm_rs{i}", [128, 512], F32), Tr()) for i in range(2)]
        tmp = [(P.sbuf(f"nm_tmp{i}", [128, 512], F32), Tr()) for i in range(3)]
        shq = 0 if which == 0 else 3
        for ti, (s, w) in enumerate(TILES):
            col = 0 if ti < 4 else 1
            if tile_pre is not None:
                tile_pre(ti)
            sqt, sqt_t = sq[0]
            pst, pst_t = ssp[ti % 2]
            rst, rst_t = rs[ti % 2]
            for c in range(8):
                P.act(sqt[:, c, :w], hget(ti, c, s, w), AF.Square, [hT_t[ti]], [sqt_t])
            for c in range(8):
                P.mm(pst[:, :w], self.onesb[:], sqt[:, c, :w], c == 0, c == 7, [self.onesb_t, sqt_t], [pst_t])
            P.act(rst[:, :w], pst[:, :w], AF.Sqrt, [pst_t], [rst_t], bias=EPS, scale=1.0 / D)
            P.op("dve", lambda e, a=rst[:, :w]: e.reciprocal(a, a), [rst_t], [rst_t])
            for c in range(8):
                tt_, tt_t = tmp[c % 3]
                P.stt("dve", tt_[:, :w], hget(ti, c, s, w), self.AB[:, which, c, col:col + 1], rst[:, :w],
                      ALU.mult, ALU.mult, [hT_t[ti], self.AB_t, rst_t], [tt_t])
                if f32cb is not None:
                    f32cb(ti, c, s, w, tt_, tt_t, self.mod_ap(shq, c, col))
                P.act(uT[:, c, s:s + w], tt_[:, :w], AF.Identity, [tt_t, self.mod_t], [uT_t[ti]],
                      bias=self.mod_ap(shq, c, col))
            if tile_cb is not None:
                tile_cb(ti, s, w)

    def moe(self, l, hT, hT_t, vT, vT_t, n_exp=NE):
        P = self.P
        moe_mark = P.mark()
        wr_d = self.inp(f"wr{l}", [128, 8 * NE])
        br_d = self.inp(f"br{l}", [128, NE])
        bb_d = self.inp(f"bexp{l}", [128, NE * 24])
        wr = P.sbuf("wr", [128, 8, NE], F32)
        wr_t = Tr()
        P.dma("sp", wr[:].rearrange("p c e -> p (c e)"), wr_d[:, :], [], [wr_t])
        br = P.sbuf("br", [128, NE], F32)
        br_t = Tr()
        P.dma("sp", br[:], br_d[:, :], [], [br_t])
        bb = P.sbuf("bexp", [128, NE, 24], F32, mid=True)
        bb_t = Tr()
        P.dma("sp", bb[:].rearrange("p e c -> p (e c)"), bb_d[:, :], [], [bb_t])
        bsig = P.sbuf("bsig", [128, NE, 8], F32, mid=True)
        bsig_t = Tr()
        bl1 = P.sbuf("bl1", [128, NE, 8], F32, mid=True)
        bl1_t = Tr()
        P.ts("dve", bsig[:], bb[:, :, 0:8], 1.702, None, ALU.mult, None, [bb_t], [bsig_t])
        P.ts("dve", bl1[:], bb[:, :, 8:16], 1.0, None, ALU.add, None, [bb_t], [bl1_t])
        sel = P.sbuf("sel", [NE, NE, 128], BF16, mid=True)
        sel_t = Tr()
        P.memset("dve", sel[:], 0.0, [sel_t])
        P.op("pool", lambda e: e.affine_select(sel[:], sel[:], [[-1, NE], [0, 128]], ALU.not_equal, 1.0, base=0,
                                               channel_multiplier=1), [sel_t], [sel_t])
        vf = [(P.sbuf(f"vf{i}", [128, 8, 512], F32), Tr()) for i in range(1)]
        lg_ps = [(P.psum(f"lgps{i}", [128, NE], F32), Tr(ps=True)) for i in range(2)]
        gT = P.sbuf("gT", [NE, T], BF16, mid=True)
        gT_t = [Tr() for _ in TILES]
        gt_ps = [(P.psum(f"gtps{i}", [NE, 128], F32), Tr(ps=True)) for i in range(2)]
        sm = {}
        for nm, shp in (("lg", [128, NE]), ("mx", [128, 8]), ("nmx", [128, 1]), ("ex", [128, NE]),
                        ("mk", [128, NE]), ("sm", [128, 1]), ("g", [128, NE])):
            sm[nm] = [(P.sbuf(f"r_{nm}{i}", shp, F32), Tr()) for i in range(2)]
        cnt = [0]
        import os
        dbg = int(os.environ.get("MOE_DBG", "9"))

        def f32cb(ti, c, s, w, tt_, tt_t, bias_ap):
            vft, vft_t = vf[0]
            P.act(vft[:, c, :w], tt_[:, :w], AF.Identity, [tt_t, self.mod_t], [vft_t], bias=bias_ap)

        def tile_cb(ti, s, w):
            vft, vft_t = vf[0]
            for b0 in range(0, w, 128):
                bw = min(128, w - b0)
                i = cnt[0] % 2
                cnt[0] += 1
                lp, lp_t = lg_ps[i]
                for c in range(8):
                    P.mm(lp[:bw, :NE], vft[:, c, b0:b0 + bw], wr[:, c, :], c == 0, c == 7, [vft_t, wr_t], [lp_t])
                lg, lg_t = sm["lg"][i]
                mx, mx_t = sm["mx"][i]
                nmx, nmx_t = sm["nmx"][i]
                ex, ex_t = sm["ex"][i]
                mk, mk_t = sm["mk"][i]
                su, su_t = sm["sm"][i]
                g, g_t = sm["g"][i]
                P.tt("dve", lg[:bw, :], lp[:bw, :NE], br[:bw, :], ALU.add, [lp_t, br_t], [lg_t])
                if dbg == 3:
                    continue
                P.op("dve", lambda e, o=mx[:bw, :], a=lg[:bw, :]: e.max(out=o, in_=a), [lg_t], [mx_t])
                P.ts("dve", nmx[:bw, :], mx[:bw, 0:1], -1.0, None, ALU.mult, None, [mx_t], [nmx_t])
                P.act(ex[:bw, :], lg[:bw, :], AF.Exp, [lg_t, nmx_t], [ex_t], bias=nmx[:bw, 0:1], scale=1.0)
                P.ts("dve", mk[:bw, :], lg[:bw, :], mx[:bw, 3:4], None, ALU.is_ge, None, [lg_t, mx_t], [mk_t])
                P.tt("dve", ex[:bw, :], ex[:bw, :], mk[:bw, :], ALU.mult, [ex_t, mk_t], [ex_t])
                P.op("dve", lambda e, o=su[:bw, :], a=ex[:bw, :]: e.reduce_sum(o, a, AX.X), [ex_t], [su_t])
                P.op("dve", lambda e, a=su[:bw, :]: e.reciprocal(a, a), [su_t], [su_t])
                P.ts("dve", g[:bw, :], ex[:bw, :], su[:bw, 0:1], None, ALU.mult, None, [ex_t, su_t], [g_t])
                if dbg == 4:
                    continue
                gp, gp_t = gt_ps[i]
                P.tp(gp[:NE, :bw], g[:bw, :], self.identf[:bw, :bw], [g_t, self.identf_t], [gp_t])
                P.copy("act", gT[:, s + b0:s + b0 + bw], gp[:NE, :bw], [gp_t], [gT_t[ti]])

        if dbg == 1:
            P.flush()
            return
        self.norm_mod(hT, hT_t, vT, vT_t, 1, f32cb=f32cb if dbg >= 2 else None, tile_cb=tile_cb if dbg >= 3 else None)
        P.flush()
        if os.environ.get("MOE_STOP") == "1":
            return
        w1_d = self.inp(f"w1_{l}", [n_exp, D, 2 * D])
        w2_d = self.inp(f"w2_{l}", [n_exp, D, D])
        w1h = [P.sbuf(f"w1h{i}", [128, 8, D], BF16) for i in range(2)]
        w1h_t = [Tr(), Tr()]
        w2b = P.sbuf("w2b", [128, 8, D], BF16)
        w2_t = Tr()
        ym = [(P.sbuf(f"ym{i}", [128, 8, 512], BF16), Tr()) for i in range(2)]
        psg = [(P.psum(f"psg{i}", [128, 512], F32), Tr(ps=True)) for i in range(2)]
        psl = [(P.psum(f"psl{i}", [128, 512], F32), Tr(ps=True)) for i in range(2)]
        psy = [(P.psum(f"psy{i}", [128, 512], F32), Tr(ps=True)) for i in range(2)]
        psb = [(P.psum(f"psb{i}", [128, 512], F32), Tr(ps=True)) for i in range(1)]
        gB = [(P.sbuf(f"gB{i}", [128, 512], F32), Tr()) for i in range(2)]
        xg = [(P.sbuf(f"xg{i}", [128, 512], F32), Tr()) for i in range(2)]
        sg = [(P.sbuf(f"sg{i}", [128, 512], F32), Tr()) for i in range(2)]
        xa = [(P.sbuf(f"xa{i}", [128, 512], F32), Tr()) for i in range(2)]
        tm = [(P.sbuf(f"tm{i}", [128, 512], F32), Tr()) for i in range(2)]
        it = 0
        jj = 0
        cc = 0
        for e_ in range(n_exp):
            w1v = w1_d[e_].rearrange("(k p) f -> p k f", p=128)
            for hf in range(2):
                P.dma("pool", w1h[hf][:, :, 0:512], w1v[:, :, hf * 512:(hf + 1) * 512], [], [w1h_t[hf]])
                P.dma("pool", w1h[hf][:, :, 512:1024], w1v[:, :, D + hf * 512:D + (hf + 1) * 512], [], [w1h_t[hf]])
            P.dma("pool", w2b[:], w2_d[e_].rearrange("(k p) f -> p k f", p=128), [], [w2_t])
            xd = int(os.environ.get("EXP_DBG", "9"))
            for ti in (4, 0, 1, 2, 3):
                s, w = TILES[ti]
                if xd == 1:
                    break
                ymt, ymt_t = ym[it % 2]
                gBt, gBt_t = gB[it % 2]
                it += 1
                pb, pb_t = psb[0]
                P.mm(pb[:, :w], sel[:, e_, :], gT[:, s:s + w], True, True, [sel_t, gT_t[ti]], [pb_t])
                P.copy("act", gBt[:, :w], pb[:, :w], [pb_t], [gBt_t])
                if xd == 2:
                    continue
                for j in range(8):
                    pg, pg_t = psg[jj % 2]
                    pl, pl_t = psl[jj % 2]
                    xgt, xgt_t = xg[jj % 2]
                    sgt, sgt_t = sg[jj % 2]
                    xat, xat_t = xa[jj % 2]
                    jj += 1
                    wh, wh_t = w1h[j // 4], w1h_t[j // 4]
                    jc = (j % 4) * 128
                    for k in range(8):
                        P.mm(pg[:, :w], wh[:, k, jc:jc + 128], vT[:, k, s:s + w], k == 0, k == 7,
                             [wh_t, vT_t[ti]], [pg_t])
                    for k in range(8):
                        P.mm(pl[:, :w], wh[:, k, 512 + jc:512 + jc + 128], vT[:, k, s:s + w], k == 0, k == 7,
                             [wh_t, vT_t[ti]], [pl_t])
                    if xd == 3:
                        continue
                    P.act(sgt[:, :w], pg[:, :w], AF.Sigmoid, [pg_t, bsig_t], [sgt_t], bias=bsig[:, e_, j:j + 1], scale=1.702)
                    P.ts("dve", xgt[:, :w], pg[:, :w], bb[:, e_, j:j + 1], self.cst[:, 0:1], ALU.add, ALU.min, [pg_t, bb_t, self.cst_t], [xgt_t])
                    P.stt("dve", xgt[:, :w], sgt[:, :w], 0.9999933, xgt[:, :w], ALU.min, ALU.mult, [sgt_t, xgt_t], [xgt_t])
                    P.ts("dve", xat[:, :w], pl[:, :w], bl1[:, e_, j:j + 1], self.cst[:, 1:2], ALU.add, ALU.max, [pl_t, bl1_t, self.cst_t], [xat_t])
                    P.stt("dve", ymt[:, j, :w], xat[:, :w], 8.0, xgt[:, :w], ALU.min, ALU.mult, [xat_t, xgt_t], [ymt_t])
                col = 0 if ti < 4 else 1
                if xd <= 4:
                    continue
                for c in range(8):
                    py, py_t = psy[cc % 2]
                    tmt, tmt_t = tm[cc % 2]
                    cc += 1
                    for j in range(8):
                        P.mm(py[:, :w], w2b[:, j, c * 128:(c + 1) * 128], ymt[:, j, :w], j == 0, j == 7,
                             [w2_t, ymt_t], [py_t])
                    P.stt("dve", tmt[:, :w], py[:, :w], bb[:, e_, 16 + c:17 + c], gBt[:, :w], ALU.add, ALU.mult,
                          [py_t, bb_t, gBt_t], [tmt_t])
                    P.stt("dve", hT[:, c, s:s + w], tmt[:, :w], self.mod_ap(5, c, col), hT[:, c, s:s + w],
                          ALU.mult, ALU.add, [tmt_t, self.mod_t, hT_t[ti]], [hT_t[ti]])
        P.flush()
        P.release(moe_mark)

    def ab_scratch(self):
        nc = self.nc
        if hasattr(self, "KA"):
            return
        self.KA = nc.dram_tensor("KA", [8, 65, NKEY], BF16).ap()
        self.VA = nc.dram_tensor("VA", [4, NKEY, 128], BF16).ap()
        self.KB = nc.dram_tensor("KB", [4, 128, NKEY], BF16).ap()
        self.VB = nc.dram_tensor("VB", [4, NKEY, 128], BF16).ap()
        self.KR = nc.dram_tensor("KR", [65, NKEY], BF16).ap()
        self.kv_t = [Tr() for _ in range(4)]
        self.QA = nc.dram_tensor("QA", [4, 8, 65, T], BF16).ap()
        self.QBn = nc.dram_tensor("QBn", [4, 4, 128, T], BF16).ap()
        self.QBr = nc.dram_tensor("QBr", [4, 4, 65, T], BF16).ap()
        self.q_t = [Tr() for _ in range(4)]

    def ab_weights(self, j):
        P = self.P
        rot_d = self.inp("rotPT", [64, 64])
        self.rotPT = P.sbuf("rotPT", [64, 64], BF16, mid=True)
        self.rot_t = Tr()
        P.dma("pool", self.rotPT[:], rot_d[:, :], [], [self.rot_t])
        sv_d = self.inp(f"abvec{j}", [128, 8])
        self.abv = P.sbuf("abv", [128, 8], F32, mid=True)
        self.abv_t = Tr()
        P.dma("sp", self.abv[:], sv_d[:, :], [], [self.abv_t])
        self.win_mark = P.mark()
        win_d = self.inp(f"win{j}", [D, 1984])
        self.win = P.sbuf("win", [128, 8, 1984], BF16, mid=True)
        self.win_t = Tr()
        P.dma("pool", self.win[:], win_d.rearrange("(k p) f -> p k f", p=128), [], [self.win_t])

    def rope_tables(self, g):
        P = self.P
        cos_d = self.inp("rope_cos", [4, 64, T])
        sin_d = self.inp("rope_sin", [4, 64, T])
        self.COS = P.sbuf("COS", [64, T], F32)
        self.SIN = P.sbuf("SIN", [64, T], F32)
        self.cs_t = Tr()
        P.dma("sp", self.COS[:], cos_d[g], [], [self.cs_t])
        P.dma("sp", self.SIN[:], sin_d[g], [], [self.cs_t])

    def inp(self, name, shape, dtype=F32):
        if name in self.ins:
            return self.ins[name]
        t = self.nc.dram_tensor(name, list(shape), dtype, kind="ExternalInput").ap()
        self.ins[name] = t
        return t

    def rope64(self, ps, ps_t, s, w, dst, dst_t, tmp, pp, pp_t, ssq=None):
        P = self.P
        xb, xb_t = tmp["xb"].next()
        t1, t1_t = tmp["t1"].next()
        t2, t2_t = tmp["t2"].next()
        P.copy("act", xb[:64, :w], ps, [ps_t], [xb_t])
        P.mm(pp[:64, :w], self.rotPT[:, :], xb[:64, :w], True, True, [self.rot_t, xb_t], [pp_t])
        P.tt("dve", t1[:64, :w], ps, self.COS[:, s:s + w], ALU.mult, [ps_t, self.cs_t], [t1_t])
        P.tt("dve", t2[:64, :w], pp[:64, :w], self.SIN[:, s:s + w], ALU.mult, [pp_t, self.cs_t], [t2_t])
        P.tt("pool", dst, t1[:64, :w], t2[:64, :w], ALU.add, [t1_t, t2_t], [dst_t])
        if ssq is not None:
            sq, sq_t = tmp["sq"].next()
            sp, sp_t = ssq
            P.act(sq[:64, :w], dst, AF.Square, [dst_t], [sq_t])
            P.mm(sp[:, :w], self.onesb[:64, :], sq[:64, :w], True, True, [self.onesb_t, sq_t], [sp_t])

    def key_cols(self, g, ti):
        s, w = TILES[ti]
        if ti < 4:
            return CTX + g * NLAT + s
        return g * NCTX

    def ab_pre(self, j, g, uT, uT_t):
        P = self.P
        self.ab_scratch()
        wukv_d = self.inp(f"wukv{j}", [128, 1024])
        wukv = P.sbuf("wukv", [128, 1024], BF16)
        wukv_t = Tr()
        P.dma("pool", wukv[:], wukv_d[:, :], [], [wukv_t])
        self.rope_tables(g)
        mk = lambda nm, shp, dt, n: Rot([(P.sbuf(f"{nm}{i}", shp, dt), Tr()) for i in range(n)])
        tmp = {"xb": mk("xb", [64, 512], BF16, 2), "t1": mk("t1", [64, 512], F32, 2), "t2": mk("t2", [64, 512], F32, 2),
               "sq": mk("sq", [128, 512], BF16, 2)}
        kst = mk("kst", [65, 512], BF16, 3)
        vst = mk("vst", [128, 512], BF16, 2)
        kbst = mk("kbst", [128, 512], BF16, 2)
        ckn = mk("ckn", [128, 512], BF16, 2)
        rsd = mk("rsd", [128, 512], F32, 2)
        mxt = mk("mxt", [128, 1], F32, 2)
        psA = Rot([(P.psum(), Tr(ps=True)) for _ in range(2)])
        psP = Rot([(P.psum(), Tr(ps=True)) for _ in range(1)])
        psS = Rot([(P.psum(), Tr(ps=True)) for _ in range(2)])
        psV = Rot([(P.psum(), Tr(ps=True)) for _ in range(1)])
        win, win_t = self.win, self.win_t
        kvt = self.kv_t[g]
        for ti, (s, w) in enumerate(TILES):
            kc0 = self.key_cols(g, ti)
            for ph in range(8):
                ps, ps_t = psA.next()
                for k in range(8):
                    P.mm(ps[:64, :w], win[:, k, 512 + ph * 64:512 + (ph + 1) * 64], uT[:, k, s:s + w], k == 0, k == 7,
                         [win_t, uT_t[ti]], [ps_t])
                kt, kt_t = kst.next()
                pp, pp_t = psP.next()
                sp = psS.next()
                self.rope64(ps[:64, :w], ps_t, s, w, kt[0:64, :w], kt_t, tmp, pp, pp_t, ssq=sp)
                P.memset("pool", kt[64:65, :w], 1.0, [kt_t])
                m, m_t = mxt.next()
                P.op("dve", lambda e, o=m[:, :], a=sp[0][:, :w]: e.reduce_max(o, a, AX.X), [sp[1]], [m_t])
                P.tt("dve", self.kmaxA[:, :], self.kmaxA[:, :], m[:, :], ALU.max, [self.kmaxA_t, m_t], [self.kmaxA_t])
                P.dma("sp", self.KA[ph, :, kc0:kc0 + w], kt[:, :w], [kt_t], [kvt])
            ps, ps_t = psA.next()
            for k in range(8):
                P.mm(ps[:64, :w], win[:, k, 1920:1984], uT[:, k, s:s + w], k == 0, k == 7, [win_t, uT_t[ti]], [ps_t])
            kt, kt_t = kst.next()
            pp, pp_t = psP.next()
            spr = psS.next()
            self.rope64(ps[:64, :w], ps_t, s, w, kt[0:64, :w], kt_t, tmp, pp, pp_t, ssq=spr)
            P.memset("pool", kt[64:65, :w], 1.0, [kt_t])
            m, m_t = mxt.next()
            P.op("dve", lambda e, o=m[:, :], a=spr[0][:, :w]: e.reduce_max(o, a, AX.X), [spr[1]], [m_t])
            P.tt("dve", self.kmaxB[:, 0:1], self.kmaxB[:, 0:1], m[:, :], ALU.max, [self.kmaxB_t, m_t], [self.kmaxB_t])
            P.dma("sp", self.KR[:, kc0:kc0 + w], kt[:, :w], [kt_t], [kvt])
            ps, ps_t = psA.next()
            for k in range(8):
                P.mm(ps[:, :w], win[:, k, 1792:1920], uT[:, k, s:s + w], k == 0, k == 7, [win_t, uT_t[ti]], [ps_t])
            sq, sq_t = tmp["sq"].next()
            P.act(sq[:, :w], ps[:, :w], AF.Square, [ps_t], [sq_t])
            sp, sp_t = psS.next()
            P.mm(sp[:, :w], self.onesb[:, :], sq[:, :w], True, True, [self.onesb_t, sq_t], [sp_t])
            rs, rs_t = rsd.next()
            P.act(rs[:, :w], sp[:, :w], AF.Sqrt, [sp_t], [rs_t], bias=EPS, scale=1.0 / 128)
            P.op("dve", lambda e, a=rs[:, :w]: e.reciprocal(a, a), [rs_t], [rs_t])
            ck, ck_t = ckn.next()
            P.stt("dve", ck[:, :w], ps[:, :w], self.abv[:, 2:3], rs[:, :w], ALU.mult, ALU.mult, [ps_t, self.abv_t, rs_t], [ck_t])
            for h in range(4):
                pk, pk_t = psA.next()
                P.mm(pk[:, :w], wukv[:, h * 256:h * 256 + 128], ck[:, :w], True, True, [wukv_t, ck_t], [pk_t])
                kb, kb_t = kbst.next()
                P.copy("act", kb[:, :w], pk[:, :w], [pk_t], [kb_t])
                sq, sq_t = tmp["sq"].next()
                P.act(sq[:, :w], kb[:, :w], AF.Square, [kb_t], [sq_t])
                sp, sp_t = psS.next()
                P.mm(sp[:, :w], self.onesb[:, :], sq[:, :w], True, True, [self.onesb_t, sq_t], [sp_t])
                m, m_t = mxt.next()
                P.op("dve", lambda e, o=m[:, :], a=sp[:, :w]: e.reduce_max(o, a, AX.X), [sp_t], [m_t])
                P.tt("dve", self.kmaxB[:, 1:2], self.kmaxB[:, 1:2], m[:, :], ALU.max, [self.kmaxB_t, m_t], [self.kmaxB_t])
                P.dma("sp", self.KB[h, :, kc0:kc0 + w], kb[:, :w], [kb_t], [kvt])
            for b0 in range(0, w, 128):
                bw = min(128, w - b0)
                pv, pv_t = psV.next()
                for k in range(8):
                    P.mm(pv[:bw, :], uT[:, k, s + b0:s + b0 + bw], win[:, k, 1024:1536], k == 0, k == 7,
                         [uT_t[ti], win_t], [pv_t])
                vt, vt_t = vst.next()
                P.copy("act", vt[:bw, :], pv[:bw, :], [pv_t], [vt_t])
                P.dma("sp", self.VA[:, kc0 + b0:kc0 + b0 + bw, :].rearrange("h k d -> k h d"),
                      vt[:bw, :].rearrange("k (h d) -> k h d", h=4), [vt_t], [kvt])
                pv, pv_t = psV.next()
                for h in range(4):
                    P.mm(pv[:bw, h * 128:(h + 1) * 128], ck[:, b0:b0 + bw], wukv[:, h * 256 + 128:h * 256 + 256], True, True,
                         [ck_t, wukv_t], [pv_t])
                vt, vt_t = vst.next()
                P.copy("dve", vt[:bw, :], pv[:bw, :], [pv_t], [vt_t])
                P.dma("sp", self.VB[:, kc0 + b0:kc0 + b0 + bw, :].rearrange("h k d -> k h d"),
                      vt[:bw, :].rearrange("k (h d) -> k h d", h=4), [vt_t], [kvt])
        self.ab_q(j, g, uT, uT_t, {"tmp": tmp, "psA": psA, "psP": psP, "psS": psS, "kst": kst, "rsd": rsd})

    def ab_layer_init(self):
        P = self.P
        P.memset("dve", self.kmaxA[:, :], 0.0, [self.kmaxA_t])
        P.memset("dve", self.kmaxB[:, :], 0.0, [self.kmaxB_t])

    def ab_q(self, j, g, uT, uT_t, shared):
        P = self.P
        tmp, psA, psP, psS, kst, rsd = (shared[k] for k in ("tmp", "psA", "psP", "psS", "kst", "rsd"))
        win, win_t = self.win, self.win_t
        wuq_d = self.inp(f"wuq{j}", [256, 768])
        wuq = P.sbuf("wuq", [128, 2, 768], BF16)
        wuq_t = Tr()
        P.dma("pool", wuq[:], wuq_d.rearrange("(c p) f -> p c f", p=128), [], [wuq_t])
        mk = lambda nm, shp, dt, n: Rot([(P.sbuf(f"{nm}{i}", shp, dt), Tr()) for i in range(n)])
        cqf = mk("cqf", [128, 2, 512], F32, 1)
        cqn = mk("cqn", [128, 2, 512], BF16, 2)
        qnb = mk("qnb", [128, 512], BF16, 2)
        axr = mk("axr", [65, 512], F32, 2)
        qt = self.q_t[g]
        for ti, (s, w) in enumerate(TILES):
            for ph in range(8):
                ps, ps_t = psA.next()
                for k in range(8):
                    P.mm(ps[:64, :w], win[:, k, ph * 64:(ph + 1) * 64], uT[:, k, s:s + w], k == 0, k == 7,
                         [win_t, uT_t[ti]], [ps_t])
                kt, kt_t = kst.next()
                pp, pp_t = psP.next()
                sp = psS.next()
                self.rope64(ps[:64, :w], ps_t, s, w, kt[0:64, :w], kt_t, tmp, pp, pp_t, ssq=sp)
                ax, ax_t = axr.next()
                P.act(ax[64:65, :w], sp[0][64:65, :w], AF.Sqrt, [sp[1]], [ax_t])
                P.copy("dve", kt[64:65, :w], ax[64:65, :w], [ax_t], [kt_t])
                P.dma("sp", self.QA[g, ph, :, s:s + w], kt[:, :w], [kt_t], [qt])
            cf, cf_t = cqf.next()
            sp, sp_t = psS.next()
            for cc in range(2):
                ps, ps_t = psA.next()
                for k in range(8):
                    P.mm(ps[:, :w], win[:, k, 1536 + cc * 128:1536 + (cc + 1) * 128], uT[:, k, s:s + w], k == 0, k == 7,
                         [win_t, uT_t[ti]], [ps_t])
                P.copy("dve", cf[:, cc, :w], ps[:, :w], [ps_t], [cf_t])
                sq, sq_t = tmp["sq"].next()
                P.act(sq[:, :w], cf[:, cc, :w], AF.Square, [cf_t], [sq_t])
                P.mm(sp[:, :w], self.onesb[:, :], sq[:, :w], cc == 0, cc == 1, [self.onesb_t, sq_t], [sp_t])
            rs, rs_t = rsd.next()
            P.act(rs[:, :w], sp[:, :w], AF.Sqrt, [sp_t], [rs_t], bias=EPS, scale=1.0 / 256)
            P.op("dve", lambda e, a=rs[:, :w]: e.reciprocal(a, a), [rs_t], [rs_t])
            cn, cn_t = cqn.next()
            for cc in range(2):
                P.stt("dve", cn[:, cc, :w], cf[:, cc, :w], self.abv[:, cc:cc + 1], rs[:, :w], ALU.mult, ALU.mult,
                      [cf_t, self.abv_t, rs_t], [cn_t])
            for h in range(4):
                ps, ps_t = psA.next()
                for cc in range(2):
                    P.mm(ps[:, :w], wuq[:, cc, h * 192:h * 192 + 128], cn[:, cc, :w], cc == 0, cc == 1, [wuq_t, cn_t], [ps_t])
                qb, qb_t = qnb.next()
                P.copy("act", qb[:, :w], ps[:, :w], [ps_t], [qb_t])
                P.dma("sp", self.QBn[g, h, :, s:s + w], qb[:, :w], [qb_t], [qt])
                sq, sq_t = tmp["sq"].next()
                P.act(sq[:, :w], qb[:, :w], AF.Square, [qb_t], [sq_t])
                spn, spn_t = psS.next()
                P.mm(spn[:, :w], self.onesb[:, :], sq[:, :w], True, True, [self.onesb_t, sq_t], [spn_t])
                ps, ps_t = psA.next()
                for cc in range(2):
                    P.mm(ps[:64, :w], wuq[:, cc, h * 192 + 128:h * 192 + 192], cn[:, cc, :w], cc == 0, cc == 1,
                         [wuq_t, cn_t], [ps_t])
                kt, kt_t = kst.next()
                pp, pp_t = psP.next()
                sp2 = psS.next()
                self.rope64(ps[:64, :w], ps_t, s, w, kt[0:64, :w], kt_t, tmp, pp, pp_t, ssq=sp2)
                ax, ax_t = axr.next()
                P.copy("dve", ax[64:65, :w], spn[64:65, :w], [spn_t], [ax_t])
                P.tt("dve", ax[64:65, :w], ax[64:65, :w], sp2[0][64:65, :w], ALU.add, [ax_t, sp2[1]], [ax_t])
                P.act(ax[64:65, :w], ax[64:65, :w], AF.Sqrt, [ax_t], [ax_t])
                P.copy("dve", kt[64:65, :w], ax[64:65, :w], [ax_t], [kt_t])
                P.dma("sp", self.QBr[g, h, :, s:s + w], kt[:, :w], [kt_t], [qt])

    def ab_attn(self, j, g, oT, oT_t):
        P = self.P
        lam_init = 0.8 - 0.6 * math.exp(-0.3 * (2 * j))
        oml = 1.0 - lam_init
        A_SCALE = 64 ** -0.5
        B_SCALE = 192 ** -0.5
        mk = lambda nm, shp, dt, n: Rot([(P.sbuf(f"{nm}{i}", shp, dt), Tr()) for i in range(n)])
        bank = [(P.psum(), Tr(ps=True)) for _ in range(8)]
        psS = Rot(bank[0:4])
        psO = Rot(bank[4:6])
        psD = Rot(bank[6:8])
        psX = psS
        dl_d = self.inp(f"dlam{j}", [128, 256])
        dl = P.sbuf("dl", [128, 256], F32)
        dl_t = Tr()
        P.dma("sp", dl[:], dl_d[:, :], [], [dl_t])
        lam = P.sbuf("lam", [128, 4], F32)
        lam_t = Tr()
        pr = P.sbuf("dlp", [128, 128], F32)
        pr_t = Tr()
        P.tt("dve", pr[:, 0:64], dl[:, 0:64], dl[:, 64:128], ALU.mult, [dl_t], [pr_t])
        P.tt("dve", pr[:, 64:128], dl[:, 128:192], dl[:, 192:256], ALU.mult, [dl_t], [pr_t])
        P.op("dve", lambda e: e.reduce_sum(lam[:, 0:1], pr[:, 0:64], AX.X), [pr_t], [lam_t])
        P.op("dve", lambda e: e.reduce_sum(lam[:, 1:2], pr[:, 64:128], AX.X), [pr_t], [lam_t])
        P.act(lam[:, 0:2], lam[:, 0:2], AF.Exp, [lam_t], [lam_t])
        P.tt("dve", lam[:, 2:3], lam[:, 0:1], lam[:, 1:2], ALU.subtract, [lam_t], [lam_t])
        P.ts("dve", lam[:, 3:4], lam[:, 2:3], lam_init, -1.0, ALU.add, ALU.mult, [lam_t], [lam_t])
        nk = P.sbuf("nk", [128, 2], F32)
        nk_t = Tr()
        P.act(nk[:, 0:1], self.kmaxA[:, 0:1], AF.Sqrt, [self.kmaxA_t], [nk_t])
        kb2 = P.sbuf("kb2", [128, 1], F32)
        kb2_t = Tr()
        P.tt("dve", kb2[:, :], self.kmaxB[:, 0:1], self.kmaxB[:, 1:2], ALU.add, [self.kmaxB_t], [kb2_t])
        P.act(nk[:, 1:2], kb2[:, :], AF.Sqrt, [kb2_t], [nk_t])
        P.ts("dve", nk[:, :], nk[:, :], -1.0, None, ALU.mult, None, [nk_t], [nk_t])
        kbuf = mk("kbuf", [128, NKEY], BF16, 3)
        vbuf = mk("vbuf", [128, NKEY // 128, 128], BF16, 2)
        qaug = mk("qaug", [65, T], BF16, 3)
        qn = mk("qn", [128, T], BF16, 2)
        pT = mk("pT", [128, 512], BF16, 4)
        of = mk("of", [128, 512], F32, 3)
        rc = mk("rc", [128, 512], F32, 2)
        sqb = mk("sqb", [128, 512], BF16, 2)
        kvall = self.kv_t
        qt = self.q_t[g]

        def load_q(src, fam):
            qa, qa_t = qaug.next()
            P.dma("sp", qa[0:65, :], src, [qt], [qa_t])
            P.ts("dve", qa[64:65, :], qa[64:65, :], nk[64:65, fam:fam + 1], None, ALU.mult, None, [qa_t, nk_t], [qa_t])
            return qa, qa_t

        def attend(kparts, q_parts, v, v_t, scale, ti):
            s, w = TILES[ti]
            nkc = NKEY // 128 if ti < 4 else CTX // 128
            po, po_t = psO.next()
            pd, pd_t = psD.next()

            def scores(kc):
                pss, pss_t = psS.next()
                for i_, ((kf, k_t), (qa, q_t)) in enumerate(zip(kparts, q_parts)):
                    P.mm(pss[:, :w], kf(kc), qa, i_ == 0, i_ == len(kparts) - 1, [k_t, q_t], [pss_t])
                return pss, pss_t

            pend = [scores(kc) for kc in range(min(3, nkc))]
            for kc in range(nkc):
                pss, pss_t = pend.pop(0)
                if kc + 3 < nkc:
                    pend.append(scores(kc + 3))
                pt, pt_t = pT.next()
                P.act(pt[:, :w], pss[:, :w], AF.Exp, [pss_t], [pt_t], scale=scale)
                P.mm(po[:, :w], v[:, kc, :], pt[:, :w], kc == 0, kc == nkc - 1, [v_t, pt_t], [po_t])
                P.mm(pd[:, :w], self.onesb[:, :], pt[:, :w], kc == 0, kc == nkc - 1, [self.onesb_t, pt_t], [pd_t])
            return (po, po_t), (pd, pd_t)

        def normalize(po, pd, ti, dst, dst_t):
            s, w = TILES[ti]
            r, r_t = rc.next()
            P.op("dve", lambda e, o=r[:, :w], a=pd[0][:, :w]: e.reciprocal(o, a), [pd[1]], [r_t])
            P.tt("dve", dst, po[0][:, :w], r[:, :w], ALU.mult, [po[1], r_t], [dst_t])

        for h in range(4):
            v, v_t = vbuf.next()
            P.dma("sp", v[:], self.VA[h].rearrange("(c p) d -> p c d", p=128), kvall, [v_t])
            kk = []
            qq = []
            for mp in range(2):
                ph = 2 * h + mp
                kb, kb_t = kbuf.next()
                P.dma("sp", kb[0:65, :], self.KA[ph], kvall, [kb_t])
                kk.append((kb, kb_t))
                qq.append(load_q(self.QA[g, ph], 0))
            for ti, (s, w) in enumerate(TILES):
                o12 = []
                for mp in range(2):
                    kb, kb_t = kk[mp]
                    qa, qa_t = qq[mp]
                    po, pd = attend([(lambda kc, kb=kb: kb[0:65, kc * 128:(kc + 1) * 128], kb_t)],
                                    [(qa[0:65, s:s + w], qa_t)], v, v_t, A_SCALE, ti)
                    o, o_t = of.next()
                    normalize(po, pd, ti, o[:, :w], o_t)
                    o12.append((o, o_t))
                (o1, o1_t), (o2, o2_t) = o12
                P.stt("dve", o1[:, :w], o2[:, :w], lam[:, 3:4], o1[:, :w], ALU.mult, ALU.add, [o2_t, lam_t, o1_t], [o1_t])
                sq, sq_t = sqb.next()
                P.act(sq[:, :w], o1[:, :w], AF.Square, [o1_t], [sq_t])
                sp, sp_t = psX.next()
                P.mm(sp[:, :w], self.onesb[:, :], sq[:, :w], True, True, [self.onesb_t, sq_t], [sp_t])
                r, r_t = rc.next()
                P.act(r[:, :w], sp[:, :w], AF.Sqrt, [sp_t], [r_t], bias=EPS / (oml * oml), scale=1.0 / (128 * oml * oml))
                P.op("dve", lambda e, a=r[:, :w]: e.reciprocal(a, a), [r_t], [r_t])
                P.stt("dve", oT[:, h, s:s + w], o1[:, :w], self.abv[:, 3:4], r[:, :w], ALU.mult, ALU.mult,
                      [o1_t, self.abv_t, r_t], [oT_t[ti]])
        kr, kr_t = kbuf.next()
        P.dma("sp", kr[0:65, :], self.KR[:, :], kvall, [kr_t])
        for h in range(4):
            v, v_t = vbuf.next()
            P.dma("sp", v[:], self.VB[h].rearrange("(c p) d -> p c d", p=128), kvall, [v_t])
            kb, kb_t = kbuf.next()
            if kb is kr:
                kb, kb_t = kbuf.next()
            P.dma("sp", kb[:, :], self.KB[h], kvall, [kb_t])
            qa, qa_t = load_q(self.QBr[g, h], 1)
            qnt, qnt_t = qn.next()
            P.dma("sp", qnt[:, :], self.QBn[g, h], [qt], [qnt_t])
            for ti, (s, w) in enumerate(TILES):
                po, pd = attend([(lambda kc, kb=kb: kb[:, kc * 128:(kc + 1) * 128], kb_t),
                                 (lambda kc, kr=kr: kr[0:65, kc * 128:(kc + 1) * 128], kr_t)],
                                [(qnt[:, s:s + w], qnt_t), (qa[0:65, s:s + w], qa_t)], v, v_t, B_SCALE, ti)
                normalize(po, pd, ti, oT[:, 4 + h, s:s + w], oT_t[ti])

    def out_proj(self, wname, hT, hT_t, oT, oT_t):
        P = self.P
        wo_d = self.inp(wname, [D, D])
        wo = P.sbuf("wo", [128, 8, D], BF16)
        wo_t = Tr()
        P.dma("pool", wo[:], wo_d.rearrange("(k p) f -> p k f", p=128), [], [wo_t])
        psY = Rot([(P.psum(), Tr(ps=True)) for _ in range(3)])
        for ti, (s, w) in enumerate(TILES):
            col = 0 if ti < 4 else 1
            for c in range(8):
                py, py_t = psY.next()
                for k in range(8):
                    P.mm(py[:, :w], wo[:, k, c * 128:(c + 1) * 128], oT[:, k, s:s + w], k == 0, k == 7, [wo_t, oT_t[ti]], [py_t])
                P.stt("dve", hT[:, c, s:s + w], py[:, :w], self.mod_ap(2, c, col), hT[:, c, s:s + w], ALU.mult, ALU.add,
                      [py_t, self.mod_t, hT_t[ti]], [hT_t[ti]])

    def h_scratch(self):
        if not hasattr(self, "H"):
            self.H = self.nc.dram_tensor("Hs", [4, D, T], F32).ap()
            self.H_t = [Tr() for _ in range(4)]
            self.xT = self.inp("xT", [4, D, T])

    def load_h(self, src, src_t, hT, hT_t):
        hv = src.rearrange("(c p) t -> p c t", p=128)
        for ti, (s, w) in enumerate(TILES):
            self.P.dma("sp", hT[:, :, s:s + w], hv[:, :, s:s + w], src_t, [hT_t[ti]])

    def store_h(self, dst, dst_t, hT, hT_t):
        hv = dst.rearrange("(c p) t -> p c t", p=128)
        for ti, (s, w) in enumerate(TILES):
            self.P.dma("sp", hv[:, :, s:s + w], hT[:, :, s:s + w], [hT_t[ti]], dst_t)

    def norm_stream(self, src, src_t, uT, uT_t, which):
        P = self.P
        hb = [P.sbuf(f"hstr{i}", [128, 8, 512], F32) for i in range(2)]
        hb_t = [Tr(), Tr()]
        hv = src.rearrange("(c p) t -> p c t", p=128)

        def issue(ti):
            s, w = TILES[ti]
            P.dma("sp", hb[ti % 2][:, :, :w], hv[:, :, s:s + w], src_t, [hb_t[ti % 2]])

        def tile_pre(ti):
            if ti == 0:
                issue(0)
            if ti + 1 < len(TILES):
                issue(ti + 1)

        self.norm_mod(None, [hb_t[ti % 2] for ti in range(len(TILES))], uT, uT_t, which,
                      hget=lambda ti, c, s, w: hb[ti % 2][:, c, :w], tile_pre=tile_pre)

    def h_src(self, l, g):
        self.h_scratch()
        if l == 0:
            return self.xT[g], []
        return self.H[g], [self.H_t[g]]

    def ab_layer(self, l, groups=(0, 1, 2, 3), attn_groups=(0, 1, 2, 3), do_moe=True, n_exp=NE):
        P = self.P
        j = l // 2
        self.h_scratch()
        self.mods(l)
        self.ab_layer_init()
        P.flush()
        m0 = P.mark()
        self.ab_weights(j)
        for g in groups:
            uT = P.sbuf("uT", [128, 8, T], BF16)
            uT_t = [Tr() for _ in TILES]
            src, src_t = self.h_src(l, g)
            self.norm_stream(src, src_t, uT, uT_t, 0)
            self.ab_pre(j, g, uT, uT_t)
            P.flush()
        P.release(self.win_mark)
        for g in attn_groups:
            m1 = P.mark()
            oT = P.sbuf("oT", [128, 8, T], BF16, mid=True)
            oT_t = [Tr() for _ in TILES]
            self.ab_attn(j, g, oT, oT_t)
            P.flush()
            hT = P.sbuf("hT", [128, 8, T], F32, mid=True)
            hT_t = [Tr() for _ in TILES]
            src, src_t = self.h_src(l, g)
            self.load_h(src, src_t, hT, hT_t)
            self.out_proj(f"woutab{j}", hT, hT_t, oT, oT_t)
            P.flush()
            if do_moe:
                vT_t = [Tr() for _ in TILES]
                self.moe(l, hT, hT_t, oT, vT_t, n_exp=n_exp)
            self.store_h(self.H[g], [self.H_t[g]], hT, hT_t)
            P.flush()
            P.release(m1)
        P.release(m0)


def _pm(v):
    return np.ascontiguousarray(np.asarray(v, np.float32).reshape(-1, 128).T)


def host_common(c_b, c_ctx):
    rotP = np.zeros((64, 64), np.float32)
    for i in list(range(0, 16)) + list(range(32, 48)):
        rotP[i, i + 16] = -1.0
        rotP[i + 16, i] = 1.0
    inv = (10000.0 ** (-np.arange(16, dtype=np.float32) / 16)).astype(np.float32)
    cos = np.ones((4, 64, T), np.float32)
    sin = np.zeros((4, 64, T), np.float32)
    for g in range(4):
        t = g * NLAT + np.arange(NLAT)
        row = (t // 64).astype(np.float32)
        col = (t % 64).astype(np.float32)
        ar = (row[None, :] * inv[:, None]).astype(np.float32)
        ac = (col[None, :] * inv[:, None]).astype(np.float32)
        cos[g, 0:16, :NLAT] = np.cos(ar)
        cos[g, 16:32, :NLAT] = np.cos(ar)
        cos[g, 32:48, :NLAT] = np.cos(ac)
        cos[g, 48:64, :NLAT] = np.cos(ac)
        sin[g, 0:16, :NLAT] = np.sin(ar)
        sin[g, 16:32, :NLAT] = np.sin(ar)
        sin[g, 32:48, :NLAT] = np.sin(ac)
        sin[g, 48:64, :NLAT] = np.sin(ac)
    return {
        "ident": np.eye(128, dtype=np.float32),
        "cT": np.ascontiguousarray(np.stack([_pm(c_b), _pm(c_ctx)], -1).reshape(128, 16)),
        "rotPT": np.ascontiguousarray(rotP.T),
        "rope_cos": cos,
        "rope_sin": sin,
    }


def host_layer_vec(l, wada, bada, gmix, gffn):
    return {f"wada{l}": np.ascontiguousarray(wada, np.float32),
            f"vec{l}": np.ascontiguousarray(np.concatenate([_pm(bada), _pm(gmix), _pm(gffn)], 1))}


def host_ab(j, win, dlam, subln, gq, gkv, wuq, wukv, wout, lam_init=None):
    abv = np.zeros((128, 8), np.float32)
    abv[:, 0] = gq[:128]
    abv[:, 1] = gq[128:]
    abv[:, 2] = gkv
    abv[:, 3] = subln
    return {f"win{j}": np.ascontiguousarray(win, np.float32), f"abvec{j}": abv,
            f"dlam{j}": np.ascontiguousarray(np.tile(np.asarray(dlam, np.float32).reshape(1, 256), (128, 1))),
            f"wuq{j}": np.ascontiguousarray(wuq, np.float32), f"wukv{j}": np.ascontiguousarray(wukv, np.float32),
            f"woutab{j}": np.ascontiguousarray(wout, np.float32)}


def host_groups(x_b, ctx_b):
    out = np.empty((4, D, T), np.float32)
    for g in range(4):
        out[g, :, :NLAT] = x_b[g * NLAT:(g + 1) * NLAT].T
        out[g, :, NLAT:] = ctx_b[g * NCTX:(g + 1) * NCTX].T
    return out


def host_moe(l, wr, br, w1, b1, w2, b2, n_exp=NE):
    ne = b1.shape[0]
    w1 = w1[:n_exp]
    w2 = w2[:n_exp]
    b1g = b1[:, 0::2].reshape(ne, 8, 128).transpose(2, 0, 1)
    b1l = b1[:, 1::2].reshape(ne, 8, 128).transpose(2, 0, 1)
    b2t = b2.reshape(ne, 8, 128).transpose(2, 0, 1)
    return {f"wr{l}": np.ascontiguousarray(wr.reshape(8, 128, NE).transpose(1, 0, 2).reshape(128, 8 * NE)),
            f"br{l}": np.ascontiguousarray(np.tile(br[None, :], (128, 1))),
            f"w1_{l}": np.ascontiguousarray(np.concatenate([w1[:, :, 0::2], w1[:, :, 1::2]], -1)),
            f"w2_{l}": np.ascontiguousarray(w2),
            f"bexp{l}": np.ascontiguousarray(np.concatenate([b1g, b1l, b2t], -1).reshape(128, ne * 24)).astype(np.float32)}


def host_c(j, l, winc, lb_raw, hg, woutc):
    return {f"winc{j}": np.ascontiguousarray(winc, np.float32),
            "lbraw": np.ascontiguousarray(np.tile(np.asarray(lb_raw, np.float32).reshape(1, 4 * D), (128, 1))),
            f"hgn{j}": np.ascontiguousarray(np.asarray(hg, np.float32).reshape(128, 1)),
            f"woutc{j}": np.ascontiguousarray(woutc, np.float32)}


def _add_c_methods():
    NCHK = NKEY // 64
    NBLK = NCHK // 4

    def c_scratch(self):
        nc = self.nc
        if hasattr(self, "cQ"):
            return
        self.cQ = nc.dram_tensor("cQ", [8, 128, NKEY], BF16).ap()
        self.cSG = nc.dram_tensor("cSG", [8, 128, NKEY], BF16).ap()
        self.cLF = [nc.dram_tensor(f"cLF{d}", [8, NKEY, 128], F32).ap() for d in range(2)]
        self.cK = [nc.dram_tensor(f"cK{d}", [8, NKEY, 128], BF16).ap() for d in range(2)]
        self.cV = nc.dram_tensor("cV", [8, NKEY, 128], BF16).ap()
        self.cO = [nc.dram_tensor(f"cO{d}", [8, 128, NKEY], F32).ap() for d in range(2)]
        self.cp_t = [Tr() for _ in range(4)]
        self.co_t = [Tr(), Tr()]

    def c_lb(self, l):
        P = self.P
        lb_d = self.inp("lbraw", [128, 4 * D])
        raw = P.sbuf("lbraw", [128, 4, D], F32)
        raw_t = Tr()
        P.dma("sp", raw[:].rearrange("p l d -> p (l d)"), lb_d[:, :], [], [raw_t])
        self.LB = P.sbuf("LBrow", [128, D], F32, mid=True)
        self.OML = P.sbuf("OMLrow", [128, D], F32, mid=True)
        self.lb_t = Tr()
        mx = P.sbuf("lbmx", [128, D], F32)
        mx_t = Tr()
        sm = P.sbuf("lbsm", [128, D], F32)
        sm_t = Tr()
        P.tt("dve", mx[:], raw[:, 0, :], raw[:, 1, :], ALU.max, [raw_t], [mx_t])
        P.tt("dve", mx[:], mx[:], raw[:, 2, :], ALU.max, [raw_t, mx_t], [mx_t])
        P.tt("dve", mx[:], mx[:], raw[:, 3, :], ALU.max, [raw_t, mx_t], [mx_t])
        for i in range(4):
            P.tt("dve", raw[:, i, :], raw[:, i, :], mx[:], ALU.subtract, [raw_t, mx_t], [raw_t])
        P.act(raw[:].rearrange("p l d -> p (l d)"), raw[:].rearrange("p l d -> p (l d)"), AF.Exp, [raw_t], [raw_t])
        P.tt("dve", sm[:], raw[:, 0, :], raw[:, 1, :], ALU.add, [raw_t], [sm_t])
        P.tt("dve", sm[:], sm[:], raw[:, 2, :], ALU.add, [raw_t, sm_t], [sm_t])
        P.tt("dve", sm[:], sm[:], raw[:, 3, :], ALU.add, [raw_t, sm_t], [sm_t])
        P.op("dve", lambda e: e.reciprocal(sm[:], sm[:]), [sm_t], [sm_t])
        P.copy("dve", mx[:], raw[:, 1, :], [raw_t], [mx_t])
        for i in range(2, l + 1):
            P.tt("dve", mx[:], mx[:], raw[:, i, :], ALU.add, [raw_t, mx_t], [mx_t])
        P.tt("dve", self.LB[:], mx[:], sm[:], ALU.mult, [mx_t, sm_t], [self.lb_t])
        P.ts("dve", self.OML[:], self.LB[:], -1.0, 1.0, ALU.mult, ALU.add, [self.lb_t], [self.lb_t])

    def c_proj(self, j, g, uT, uT_t):
        P = self.P
        self.c_scratch()
        mk = lambda nm, shp, dt, n: Rot([(P.sbuf(f"{nm}{i}", shp, dt), Tr()) for i in range(n)])
        wq_d = self.inp(f"winc{j}", [D, 5 * D])
        wv = wq_d.rearrange("(k p) f -> p k f", p=128)
        wbuf = mk("cw", [128, 8, 512], BF16, 2)
        ps = Rot([(P.psum(), Tr(ps=True)) for _ in range(4)])
        fm = mk("cfm", [128, 512], BF16, 3)
        sgm = mk("csg", [128, 512], F32, 2)
        fz = mk("cfz", [128, 512], F32, 2)
        lf = mk("clf", [128, 512], F32, 2)
        kk = mk("ckk", [128, 512], BF16, 2)
        vv = mk("cvv", [128, 512], BF16, 2)
        pt = self.cp_t[g]
        for piece in range(10):
            blk = piece // 2
            wt, wt_t = wbuf.next()
            P.dma("pool", wt[:], wv[:, :, piece * 512:(piece + 1) * 512], [], [wt_t])
            h0 = (piece % 2) * 4
            for ti, (s, w) in enumerate(TILES):
                kc0 = self.key_cols(g, ti)
                if blk in (0, 4):
                    for hh in range(4):
                        p_, p_t = ps.next()
                        for k in range(8):
                            P.mm(p_[:, :w], wt[:, k, hh * 128:(hh + 1) * 128], uT[:, k, s:s + w], k == 0, k == 7,
                                 [wt_t, uT_t[ti]], [p_t])
                        o_, o_t = fm.next()
                        if blk == 0:
                            P.copy("act", o_[:, :w], p_[:, :w], [p_t], [o_t])
                            P.dma("sp", self.cQ[h0 + hh, :, kc0:kc0 + w], o_[:, :w], [o_t], [pt])
                        else:
                            P.act(o_[:, :w], p_[:, :w], AF.Silu, [p_t], [o_t])
                            P.dma("sp", self.cSG[h0 + hh, :, kc0:kc0 + w], o_[:, :w], [o_t], [pt])
                else:
                    for b0 in range(0, w, 128):
                        bw = min(128, w - b0)
                        p_, p_t = ps.next()
                        for k in range(8):
                            P.mm(p_[:bw, :], uT[:, k, s + b0:s + b0 + bw], wt[:, k, :], k == 0, k == 7,
                                 [uT_t[ti], wt_t], [p_t])
                        if blk == 3:
                            v_, v_t = vv.next()
                            P.copy("act", v_[:bw, :], p_[:bw, :], [p_t], [v_t])
                            P.dma("sp", self.cV[h0:h0 + 4, kc0 + b0:kc0 + b0 + bw, :].rearrange("h k d -> k h d"),
                                  v_[:bw, :].rearrange("k (h d) -> k h d", h=4), [v_t], [pt])
                        else:
                            dr = blk - 1
                            c0 = h0 * 128
                            sg_, sg_t = sgm.next()
                            f_, f_t = fz.next()
                            l_, l_t = lf.next()
                            k_, k_t = kk.next()
                            P.act(sg_[:bw, :], p_[:bw, :], AF.Sigmoid, [p_t], [sg_t])
                            P.tt("dve", f_[:bw, :], sg_[:bw, :], self.OML[:bw, c0:c0 + 512], ALU.mult, [sg_t, self.lb_t], [f_t])
                            P.tt("dve", f_[:bw, :], f_[:bw, :], self.LB[:bw, c0:c0 + 512], ALU.add, [f_t, self.lb_t], [f_t])
                            P.act(l_[:bw, :], f_[:bw, :], AF.Ln, [f_t], [l_t])
                            P.ts("dve", k_[:bw, :], f_[:bw, :], -1.0, 1.0, ALU.mult, ALU.add, [f_t], [k_t])
                            P.dma("sp", self.cLF[dr][h0:h0 + 4, kc0 + b0:kc0 + b0 + bw, :].rearrange("h k d -> k h d"),
                                  l_[:bw, :].rearrange("k (h d) -> k h d", h=4), [l_t], [pt])
                            P.dma("sp", self.cK[dr][h0:h0 + 4, kc0 + b0:kc0 + b0 + bw, :].rearrange("h k d -> k h d"),
                                  k_[:bw, :].rearrange("k (h d) -> k h d", h=4), [k_t], [pt])

    def c_scan(self):
        P = self.P
        mk = lambda nm, shp, dt, n: Rot([(P.sbuf(f"{nm}{i}", shp, dt), Tr()) for i in range(n)])
        U = []
        Um = []
        for d_ in range(2):
            u = P.sbuf(f"U{d_}", [64, 64], F32)
            u_t = Tr()
            P.memset("dve", u[:], 1.0, [u_t])
            cm, coef = (-1, 1) if d_ == 0 else (1, -1)
            P.op("pool", lambda e, u=u, cm=cm, coef=coef: e.affine_select(u[:], u[:], [[coef, 64]], ALU.is_ge, 0.0, base=0,
                                                                          channel_multiplier=cm), [u_t], [u_t])
            U.append((u, u_t))
        S = [[(P.sbuf(f"S{d_}", [128, 128], F32), Tr()), (P.sbuf(f"Sb{d_}", [128, 128], BF16), Tr())] for d_ in range(2)]
        lfb = [mk(f"slf{d_}", [64, 4, 128], F32, 2) for d_ in range(2)]
        kb_ = [mk(f"sk{d_}", [64, 4, 128], BF16, 2) for d_ in range(2)]
        vb_ = [mk(f"sv{d_}", [64, 4, 128], BF16, 2) for d_ in range(2)]
        qb_ = [mk(f"sq{d_}", [128, 256], BF16, 2) for d_ in range(2)]
        ob_ = [mk(f"so{d_}", [128, 256], F32, 2) for d_ in range(2)]
        eq = mk("seq", [128, 64], F32, 4)
        ek = mk("sek", [64, 128], F32, 4)
        qt_ = mk("sqt", [128, 64], BF16, 4)
        kt_ = mk("skt", [64, 128], BF16, 4)
        ktT = mk("sktT", [128, 64], BF16, 4)
        at_ = mk("sat", [64, 64], BF16, 4)
        tS = mk("stS", [128, 128], F32, 2)
        banks = [(P.psum(), Tr(ps=True)) for _ in range(8)]
        pc = Rot(banks[0:2])
        pa = Rot(banks[2:4])
        po = Rot(banks[4:6])
        pd = Rot(banks[6:8])
        allp = self.cp_t
        for h in range(8):
            for d_ in range(2):
                (s_, s_t), (sb, sb_t) = S[d_]
                P.memset("dve", s_[:], 0.0, [s_t])
                P.memset("pool", sb[:], 0.0, [sb_t])
            order = [list(range(NBLK)), [0] + list(range(NBLK - 1, 0, -1))]
            for bi in range(NBLK):
                cur = []
                for d_ in range(2):
                    b = order[d_][bi]
                    c0 = b * 256
                    l_, l_t = lfb[d_].next()
                    k_, k_t = kb_[d_].next()
                    v_, v_t = vb_[d_].next()
                    q_, q_t = qb_[d_].next()
                    o_, o_t = ob_[d_].next()
                    P.dma("sp", l_[:], self.cLF[d_][h, c0:c0 + 256, :].rearrange("(c s) d -> s c d", s=64), allp, [l_t])
                    P.dma("sp", k_[:], self.cK[d_][h, c0:c0 + 256, :].rearrange("(c s) d -> s c d", s=64), allp, [k_t])
                    P.dma("sp", v_[:], self.cV[h, c0:c0 + 256, :].rearrange("(c s) d -> s c d", s=64), allp, [v_t])
                    P.dma("sp", q_[:], self.cQ[h, :, c0:c0 + 256], allp, [q_t])
                    cur.append((b, c0, l_, l_t, k_, k_t, v_, v_t, q_, q_t, o_, o_t))
                for ci in range(4):
                    for d_ in range(2):
                        b, c0, l_, l_t, k_, k_t, v_, v_t, q_, q_t, o_, o_t = cur[d_]
                        c = ci if d_ == 0 else 3 - ci
                        u, u_t = U[d_]
                        (s_, s_t), (sb, sb_t) = S[d_]
                        p1, p1_t = pc.next()
                        P.mm(p1[:, 0:64], l_[:, c, :], u[:, :], True, True, [l_t, u_t], [p1_t])
                        P.mm(p1[:64, 128:256], u[:, :], l_[:, c, :], True, True, [u_t, l_t], [p1_t])
                        e1, e1_t = eq.next()
                        e2, e2_t = ek.next()
                        P.act(e1[:, :], p1[:, 0:64], AF.Exp, [p1_t], [e1_t])
                        P.act(e2[:, :], p1[:64, 128:256], AF.Exp, [p1_t], [e2_t], scale=-1.0)
                        qt, qt_t = qt_.next()
                        kt, kt_t = kt_.next()
                        P.tt("dve", qt[:, :], q_[:, c * 64:(c + 1) * 64], e1[:, :], ALU.mult, [q_t, e1_t], [qt_t])
                        P.tt("dve", kt[:, :], k_[:, c, :], e2[:, :], ALU.mult, [k_t, e2_t], [kt_t])
                        p2, p2_t = pa.next()
                        p2b = p2[:].bitcast(BF16)
                        P.tp(p2b[:, 0:64], kt[:, :], self.identb[:64, :64], [kt_t, self.identb_t], [p2_t])
                        kT, kT_t = ktT.next()
                        P.copy("act", kT[:, :], p2b[:, 0:64], [p2_t], [kT_t])
                        P.mm(p2[:64, 256:320], kT[:, :], qt[:, :], True, True, [kT_t, qt_t], [p2_t])
                        at, at_t = at_.next()
                        P.tt("dve", at[:, :], p2[:64, 256:320], u[:, :], ALU.mult, [p2_t, u_t], [at_t])
                        p3, p3_t = po.next()
                        P.mm(p3[:, 0:64], v_[:, c, :], at[:, :], True, False, [v_t, at_t], [p3_t])
                        P.mm(p3[:, 0:64], sb[:, :], qt[:, :], False, True, [sb_t, qt_t], [p3_t])
                        P.copy("act", o_[:, c * 64:(c + 1) * 64], p3[:, 0:64], [p3_t], [o_t])
                        p4, p4_t = pd.next()
                        P.mm(p4[:, 0:128], kt[:, :], v_[:, c, :], True, True, [kt_t, v_t], [p4_t])
                        ts_, ts_t = tS.next()
                        ecol = e1[:, 63:64] if d_ == 0 else e1[:, 0:1]
                        P.tt("dve", ts_[:, :], s_[:, :], p4[:, 0:128], ALU.add, [s_t, p4_t], [ts_t])
                        P.ts("dve", s_[:, :], ts_[:, :], ecol, None, ALU.mult, None, [ts_t, e1_t], [s_t])
                        P.copy("act", sb[:, :], s_[:, :], [s_t], [sb_t])
                for d_ in range(2):
                    b, c0, l_, l_t, k_, k_t, v_, v_t, q_, q_t, o_, o_t = cur[d_]
                    P.dma("sp", self.cO[d_][h, :, c0:c0 + 256], o_[:, :], [o_t], [self.co_t[d_]])

    def c_readout(self, j, g, oT, oT_t):
        P = self.P
        mk = lambda nm, shp, dt, n: Rot([(P.sbuf(f"{nm}{i}", shp, dt), Tr()) for i in range(n)])
        hg_d = self.inp(f"hgn{j}", [128, 1])
        hg = P.sbuf("hgn", [128, 1], F32)
        hg_t = Tr()
        P.dma("sp", hg[:], hg_d[:, :], [], [hg_t])
        of = mk("rof", [128, 512], F32, 2)
        ob = mk("rob", [128, 512], F32, 2)
        sg = mk("rsg", [128, 512], BF16, 2)
        sq = mk("rsq", [128, 512], BF16, 2)
        rs = mk("rrs", [128, 512], F32, 2)
        ps = Rot([(P.psum(), Tr(ps=True)) for _ in range(2)])
        for h in range(8):
            for ti, (s, w) in enumerate(TILES):
                kc0 = self.key_cols(g, ti)
                a, a_t = of.next()
                b, b_t = ob.next()
                c, c_t = sg.next()
                P.dma("sp", a[:, :w], self.cO[0][h, :, kc0:kc0 + w], [self.co_t[0]], [a_t])
                P.dma("sp", b[:, :w], self.cO[1][h, :, kc0:kc0 + w], [self.co_t[1]], [b_t])
                P.dma("sp", c[:, :w], self.cSG[h, :, kc0:kc0 + w], self.cp_t, [c_t])
                P.tt("dve", a[:, :w], a[:, :w], b[:, :w], ALU.add, [a_t, b_t], [a_t])
                q, q_t = sq.next()
                P.act(q[:, :w], a[:, :w], AF.Square, [a_t], [q_t])
                p, p_t = ps.next()
                P.mm(p[:, :w], self.onesb[:, :], q[:, :w], True, True, [self.onesb_t, q_t], [p_t])
                r, r_t = rs.next()
                P.act(r[:, :w], p[:, :w], AF.Sqrt, [p_t], [r_t], bias=EPS, scale=1.0 / 128)
                P.op("dve", lambda e, x=r[:, :w]: e.reciprocal(x, x), [r_t], [r_t])
                P.stt("dve", a[:, :w], a[:, :w], hg[:, 0:1], r[:, :w], ALU.mult, ALU.mult, [a_t, hg_t, r_t], [a_t])
                P.tt("dve", oT[:, h, s:s + w], a[:, :w], c[:, :w], ALU.mult, [a_t, c_t], [oT_t[ti]])

    def c_layer(self, l, groups=(0, 1, 2, 3), out_groups=(0, 1, 2, 3), do_moe=True, n_exp=NE):
        P = self.P
        j = l // 2
        self.h_scratch()
        self.c_scratch()
        self.mods(l)
        P.flush()
        m0 = P.mark()
        self.c_lb(l)
        P.flush()
        for g in groups:
            uT = P.sbuf("uT", [128, 8, T], BF16)
            uT_t = [Tr() for _ in TILES]
            src, src_t = self.h_src(l, g)
            self.norm_stream(src, src_t, uT, uT_t, 0)
            self.c_proj(j, g, uT, uT_t)
            P.flush()
        P.release(m0)
        self.c_scan()
        P.flush()
        for g in out_groups:
            m1 = P.mark()
            oT = P.sbuf("oT", [128, 8, T], BF16, mid=True)
            oT_t = [Tr() for _ in TILES]
            self.c_readout(j, g, oT, oT_t)
            P.flush()
            hT = P.sbuf("hT", [128, 8, T], F32, mid=True)
            hT_t = [Tr() for _ in TILES]
            src, src_t = self.h_src(l, g)
            self.load_h(src, src_t, hT, hT_t)
            self.out_proj(f"woutc{j}", hT, hT_t, oT, oT_t)
            P.flush()
            if do_moe:
                vT_t = [Tr() for _ in TILES]
                self.moe(l, hT, hT_t, oT, vT_t, n_exp=n_exp)
            self.store_h(self.H[g], [self.H_t[g]], hT, hT_t)
            P.flush()
            P.release(m1)

    for f in (c_scratch, c_lb, c_proj, c_scan, c_readout, c_layer):
        setattr(LB, f.__name__, f)


_add_c_methods()


def _final_norm(self, g, out_d):
    P = self.P
    fg_d = self.inp("fing", [128, 8])
    fg = P.sbuf("fing", [128, 8], F32)
    fg_t = Tr()
    P.dma("sp", fg[:], fg_d[:, :], [], [fg_t])
    hv = self.H[g].rearrange("(c p) t -> p c t", p=128)
    ov = out_d[g].rearrange("(c p) t -> p c t", p=128)
    hb = [(P.sbuf(f"fh{i}", [128, 8, 512], F32), Tr()) for i in range(2)]
    sq = [(P.sbuf(f"fsq{i}", [128, 8, 512], BF16), Tr()) for i in range(1)]
    rs = [(P.sbuf(f"frs{i}", [128, 512], F32), Tr()) for i in range(2)]
    ps = [(P.psum(), Tr(ps=True)) for _ in range(2)]
    for ti in range(4):
        s, w = TILES[ti]
        h_, h_t = hb[ti % 2]
        q_, q_t = sq[0]
        r_, r_t = rs[ti % 2]
        p_, p_t = ps[ti % 2]
        P.dma("sp", h_[:, :, :w], hv[:, :, s:s + w], [self.H_t[g]], [h_t])
        for c in range(8):
            P.act(q_[:, c, :w], h_[:, c, :w], AF.Square, [h_t], [q_t])
        for c in range(8):
            P.mm(p_[:, :w], self.onesb[:, :], q_[:, c, :w], c == 0, c == 7, [self.onesb_t, q_t], [p_t])
        P.act(r_[:, :w], p_[:, :w], AF.Sqrt, [p_t], [r_t], bias=EPS, scale=1.0 / D)
        P.op("dve", lambda e, a=r_[:, :w]: e.reciprocal(a, a), [r_t], [r_t])
        for c in range(8):
            P.stt("dve", h_[:, c, :w], h_[:, c, :w], fg[:, c:c + 1], r_[:, :w], ALU.mult, ALU.mult, [h_t, fg_t, r_t], [h_t])
        P.dma("sp", ov[:, :, s:s + w], h_[:, :, :w], [h_t], [])


LB.final_norm = _final_norm


def build_program(n_exp=NE, layers=(0, 1, 2, 3)):
    B = LB()
    P = B.P
    B.consts()
    P.flush()
    B.h_scratch()
    if 0 not in layers:
        for g in range(4):
            P.dma("sp", B.H[g], B.xT[g], [], [B.H_t[g]])
        P.flush()
    for l in layers:
        if l % 2 == 0:
            B.ab_layer(l, n_exp=n_exp)
        else:
            B.c_layer(l, n_exp=n_exp)
    out_d = B.out("outT", [4, D, NLAT])
    for g in range(4):
        B.final_norm(g, out_d)
        P.flush()
    P.close()
    return B


def host_inputs(inp, b, n_exp=NE, layers=(0, 1, 2, 3)):
    f = lambda a: np.asarray(a, np.float32)
    m = host_common(f(inp["c"])[b], f(inp["c_ctx"]))
    m["xT"] = host_groups(f(inp["x"])[b], f(inp["ctx"])[b])
    m["fing"] = _pm(f(inp["final_g"]))
    for l in layers:
        j = l // 2
        m.update(host_layer_vec(l, f(inp["w_ada"])[l], f(inp["b_ada"])[l], f(inp["norm_mix_g"])[l], f(inp["norm_ffn_g"])[l]))
        if l % 2 == 0:
            m.update(host_ab(j, f(inp["w_in_ab"])[j], f(inp["diff_lambda"])[j], f(inp["diff_subln_g"])[j],
                             f(inp["mla_q_norm_g"])[j], f(inp["mla_kv_norm_g"])[j], f(inp["w_uq"])[j], f(inp["w_ukv"])[j],
                             f(inp["w_out_ab"])[j]))
        else:
            m.update(host_c(j, l, f(inp["w_in_c"])[j], f(inp["lb_raw"]), f(inp["hgrn_norm_g"])[j], f(inp["w_out_c"])[j]))
        m.update(host_moe(l, f(inp["w_router"])[l], f(inp["b_router"])[l], inp["w_exp1"][l], f(inp["b_exp1"])[l],
                          inp["w_exp2"][l], f(inp["b_exp2"])[l], n_exp))
    return m


def kernel(**inputs):
    B = build_program()
    in_maps = []
    for b in range(2):
        m = host_inputs(inputs, b)
        in_maps.append({k: m[k] for k in B.ins})
    res = run_bass_kernel_spmd(B.nc, in_maps, core_ids=[0, 1])
    out = np.empty((2, SEQ, D), np.float32)
    for b in range(2):
        oT = res.results[b]["outT"]
        for g in range(4):
            out[b, g * NLAT:(g + 1) * NLAT, :] = oT[g].T
    return out
```

```python
import math
from contextlib import ExitStack
import numpy as np
import concourse.bass as bass
import concourse.mybir as mybir
from concourse.bass_utils import run_bass_kernel_spmd

F32 = mybir.dt.float32
BF16 = mybir.dt.bfloat16
I32 = mybir.dt.int32
AF = mybir.ActivationFunctionType
ALU = mybir.AluOpType
AX = mybir.AxisListType

D = 1024
NCH = 8
NLAT = 2048
NCTX = 64
T = NLAT + NCTX
SEQ = 8192
CTX = 256
NKEY = SEQ + CTX
DEPTH = 4
NE = 32
EPS = 1e-6
TILES = [(0, 512), (512, 512), (1024, 512), (1536, 512), (2048, 64)]

EPOCH = 30000


class Tr:
    __slots__ = ("name", "w", "rs", "rd", "ps")

    def __init__(self, name="", ps=False):
        self.name = name
        self.ps = ps
        self.w = None
        self.rs = {}
        self.rd = []


class Op:
    __slots__ = ("eng", "fn", "dma", "deps", "ddeps", "signal", "sidx", "dsem", "dval", "idx")

    def __init__(self, eng, fn, dma):
        self.eng = eng
        self.fn = fn
        self.dma = dma
        self.deps = {}
        self.ddeps = []
        self.signal = False
        self.sidx = 0
        self.dsem = None
        self.dval = 0
        self.idx = 0


ENGS = ("pe", "dve", "act", "pool", "sp")
NDMASEM = {"sp": 12, "pool": 8, "act": 4, "dve": 2, "pe": 2}


class Prog:
    def __init__(self, nc):
        self.nc = nc
        self.stack = ExitStack()
        self.left = self.SB_LO
        self.left_persist = self.SB_LO
        self.right = self.SB_HI
        self.nbank = 0
        self.banks = [self.stack.enter_context(nc.psum_tensor(f"bank{i}", [128, 512], F32)) for i in range(8)]
        self.nbuf = 0
        self.sems = {e: [] for e in ENGS}
        self.dsems = {e: [] for e in ENGS}
        self.sbase = {e: 0 for e in ENGS}
        self.dcount = {e: 0 for e in ENGS}
        self.clock = {e: {} for e in ENGS}
        self.gidx = {e: 0 for e in ENGS}
        self._reset()

    def _reset(self):
        self.ops = {e: [] for e in ENGS}
        self.dmaq = {e: [] for e in ENGS}
        self.all_dma = []

    SB_LO = 16512
    SB_HI = 229344

    def sbuf(self, name, shape, dtype, persist=False, mid=False):
        self.nbuf += 1
        n = 1
        for d_ in shape[1:]:
            n *= d_
        nbytes = n * (2 if dtype == BF16 else 4)
        nbytes = (nbytes + 63) // 64 * 64
        if persist or mid:
            off = self.left
            self.left += nbytes
        else:
            self.right -= nbytes
            off = self.right
        assert self.left <= self.right, f"SBUF overflow allocating {name}: left={self.left} right={self.right}"
        return self.nc.alloc_sbuf_tensor_at(f"{name}_{self.nbuf}", list(shape), dtype, offset=off)

    def psum(self, name=None, shape=None, dtype=None):
        assert self.nbank < 8, "out of PSUM banks"
        t = self.banks[self.nbank]
        self.nbank += 1
        return t

    def _need(self, o, p, raw):
        if p is None or p is o:
            return
        if p.dma:
            if p not in o.ddeps:
                o.ddeps.append(p)
            return
        if (not o.dma) and p.eng == o.eng:
            if not raw or o.eng == "pe":
                return
        cur = o.deps.get(p.eng)
        if cur is None or cur.idx < p.idx:
            o.deps[p.eng] = p
        p.signal = True

    def op(self, eng, fn, reads=(), writes=(), dma=False):
        o = Op(eng, fn, dma)
        self.gidx[eng] += 1
        o.idx = self.gidx[eng]
        for t in reads:
            self._need(o, t.w, True)
            if t.ps:
                for r in t.rs.values():
                    if r.eng != eng:
                        self._need(o, r, False)
        for t in writes:
            self._need(o, t.w, False)
            for r in t.rs.values():
                self._need(o, r, False)
            for r in t.rd:
                self._need(o, r, False)
        if dma:
            q = self.dmaq[eng]
            n = NDMASEM[eng]
            if len(q) >= n:
                self._need(o, q[len(q) - n], False)
            q.append(o)
            self.all_dma.append(o)
        for t in reads:
            if dma:
                t.rd.append(o)
            else:
                t.rs[eng] = o
        for t in writes:
            t.w = o
            t.rs = {}
            t.rd = []
        self.ops[eng].append(o)
        return o

    def flush(self):
        nc = self.nc
        lasts = []
        for e in ENGS:
            for o in reversed(self.ops[e]):
                if not o.dma:
                    lasts.append(o)
                    break
        for e in ENGS:
            o = Op(e, None, False)
            self.gidx[e] += 1
            o.idx = self.gidx[e]
            for p in lasts:
                if p.eng != e:
                    cur = o.deps.get(p.eng)
                    o.deps[p.eng] = p
                    p.signal = True
            o.ddeps = list(self.all_dma)
            self.ops[e].append(o)
        for e in ENGS:
            c = self.sbase[e]
            for o in self.ops[e]:
                if o.signal and not o.dma:
                    c += 1
                    o.sidx = c
            self.sbase[e] = c
            need = (c + EPOCH - 1) // EPOCH
            while len(self.sems[e]) < need:
                self.sems[e].append(self.stack.enter_context(nc.semaphore(f"s_{e}_{len(self.sems[e])}")))
        for e in ENGS:
            n = NDMASEM[e]
            if self.dmaq[e]:
                while len(self.dsems[e]) < n:
                    self.dsems[e].append(self.stack.enter_context(nc.semaphore(f"d_{e}_{len(self.dsems[e])}")))
            for o in self.dmaq[e]:
                i = self.dcount[e]
                self.dcount[e] += 1
                o.dsem = (e, i % n)
                o.dval = 16 * (i // n + 1)
        sems, dsems = self.sems, self.dsems
        self._check_deadlock()

        def run_engine(e, eng):
            clock = self.clock[e]
            for o in self.ops[e]:
                for f, p in o.deps.items():
                    if clock.get(f, 0) < p.sidx:
                        s = p.sidx - 1
                        eng.wait_ge(sems[f][s // EPOCH], s % EPOCH + 1)
                        clock[f] = p.sidx
                for p in o.ddeps:
                    key = ("d",) + p.dsem
                    if clock.get(key, 0) < p.dval:
                        eng.wait_ge(dsems[p.dsem[0]][p.dsem[1]], p.dval)
                        clock[key] = p.dval
                if o.dma:
                    key = ("d",) + o.dsem
                    if clock.get(key, 0) < o.dval - 16:
                        eng.wait_ge(dsems[o.dsem[0]][o.dsem[1]], o.dval - 16)
                        clock[key] = o.dval - 16
                if o.fn is None:
                    continue
                ins = o.fn(eng)
                if o.dma:
                    ins.then_inc(dsems[o.dsem[0]][o.dsem[1]], 16)
                elif o.signal:
                    s = o.sidx - 1
                    ins.then_inc(sems[e][s // EPOCH], 1)

        with nc.Block() as block:
            @block.sync
            def _(eng):
                run_engine("sp", eng)

            @block.tensor
            def _(eng):
                run_engine("pe", eng)

            @block.vector
            def _(eng):
                run_engine("dve", eng)

            @block.scalar
            def _(eng):
                run_engine("act", eng)

            @block.gpsimd
            def _(eng):
                run_engine("pool", eng)
        self._reset()
        self.right = self.SB_HI
        self.nbank = 0

    def _check_deadlock(self):
        cnt = dict(self._sim_cnt) if hasattr(self, "_sim_cnt") else {}
        pos = {e: 0 for e in ENGS}
        progress = True
        while progress:
            progress = False
            for e in ENGS:
                ops = self.ops[e]
                while pos[e] < len(ops):
                    o = ops[pos[e]]
                    ok = True
                    for f, p in o.deps.items():
                        if p.sidx and cnt.get(("s", f), 0) < p.sidx:
                            ok = False
                    for p in o.ddeps:
                        if cnt.get(("d",) + p.dsem, 0) < p.dval:
                            ok = False
                    if o.dma and cnt.get(("d",) + o.dsem, 0) < o.dval - 16:
                        ok = False
                    if not ok:
                        break
                    if o.dma:
                        cnt[("d",) + o.dsem] = cnt.get(("d",) + o.dsem, 0) + 16
                    elif o.signal:
                        assert cnt.get(("s", e), 0) == o.sidx - 1, (e, cnt.get(("s", e), 0), o.sidx)
                        cnt[("s", e)] = o.sidx
                    pos[e] += 1
                    progress = True
        for e in ENGS:
            assert pos[e] == len(self.ops[e]), f"DEADLOCK: engine {e} stuck at op {pos[e]}/{len(self.ops[e])}"
        self._sim_cnt = cnt

    def mark(self):
        return self.left

    def release(self, m):
        self.left = m

    def close(self):
        self.stack.close()

    def mm(self, out, lhsT, rhs, start, stop, reads, writes):
        return self.op("pe", lambda e: e.matmul(out, lhsT, rhs, start=start, stop=stop), reads, writes)

    def tp(self, out, in_, ident, reads, writes):
        return self.op("pe", lambda e: e.transpose(out, in_, ident), reads, writes)

    def dma(self, eng, out, in_, reads, writes):
        return self.op(eng, lambda e: e.dma_start(out=out, in_=in_), reads, writes, dma=True)

    def act(self, out, in_, func, reads, writes, bias=None, scale=None):
        kw = {}
        if bias is not None:
            kw["bias"] = bias
        if scale is not None:
            kw["scale"] = scale
        return self.op("act", lambda e: e.activation(out, in_, func, **kw), reads, writes)

    def ts(self, eng, out, in0, s1, s2, op0, op1, reads, writes):
        if op1 is None:
            return self.op(eng, lambda e: e.tensor_scalar(out, in0, s1, None, op0), reads, writes)
        return self.op(eng, lambda e: e.tensor_scalar(out, in0, s1, s2, op0, op1), reads, writes)

    def tt(self, eng, out, in0, in1, op, reads, writes):
        return self.op(eng, lambda e: e.tensor_tensor(out, in0, in1, op), reads, writes)

    def stt(self, eng, out, in0, scalar, in1, op0, op1, reads, writes):
        return self.op(eng, lambda e: e.scalar_tensor_tensor(out, in0, scalar, in1, op0, op1), reads, writes)

    def copy(self, eng, out, in_, reads, writes):
        if eng == "act":
            return self.op("act", lambda e: e.copy(out, in_), reads, writes)
        return self.op(eng, lambda e: e.tensor_copy(out, in_), reads, writes)

    def memset(self, eng, ap, val, writes):
        return self.op(eng, lambda e: e.memset(ap, val), (), writes)


class Rot:
    def __init__(self, items):
        self.items = items
        self.i = 0

    def next(self):
        it = self.items[self.i % len(self.items)]
        self.i += 1
        return it


class LB:
    def __init__(self):
        self.nc = bass.Bass("TRN2", target_bir_lowering=False)
        self.P = Prog(self.nc)
        self.ins = {}
        self.outs = {}

    def inp(self, name, shape, dtype=F32):
        t = self.nc.dram_tensor(name, list(shape), dtype, kind="ExternalInput").ap()
        self.ins[name] = t
        return t

    def out(self, name, shape, dtype=F32):
        t = self.nc.dram_tensor(name, list(shape), dtype, kind="ExternalOutput").ap()
        self.outs[name] = t
        return t

    def consts(self):
        P = self.P
        ident_d = self.inp("ident", [128, 128])
        self.identf = P.sbuf("identf", [128, 128], F32, persist=True)
        self.identf_t = Tr()
        self.identb = P.sbuf("identb", [128, 128], BF16, persist=True)
        self.identb_t = Tr()
        self.onesb = P.sbuf("onesb", [128, 128], BF16, persist=True)
        self.onesb_t = Tr()
        P.dma("sp", self.identf[:], ident_d[:, :], [], [self.identf_t])
        P.dma("pool", self.identb[:], ident_d[:, :], [], [self.identb_t])
        P.memset("dve", self.onesb[:], 1.0, [self.onesb_t])
        self.cst = P.sbuf("cst", [128, 4], F32, persist=True)
        self.cst_t = Tr()
        for i_, v_ in enumerate((7.0, -6.0, 8.0, 1.0)):
            P.memset("dve", self.cst[:, i_:i_ + 1], v_, [self.cst_t])
        self.kmaxA = P.sbuf("kmaxA", [128, 1], F32, persist=True)
        self.kmaxB = P.sbuf("kmaxB", [128, 2], F32, persist=True)
        self.kmaxA_t = Tr()
        self.kmaxB_t = Tr()
        cT_d = self.inp("cT", [128, 16])
        self.scT = P.sbuf("scT", [128, 8, 2], F32, persist=True)
        self.scT_t = Tr()
        cT = P.sbuf("cTf", [128, 16], F32)
        cT_t = Tr()
        P.dma("sp", cT[:], cT_d[:, :], [], [cT_t])
        P.act(self.scT[:].rearrange("p c n -> p (c n)"), cT[:], AF.Silu, [cT_t], [self.scT_t])

    def mods(self, l):
        P = self.P
        wada = self.inp(f"wada{l}", [D, 6 * D])
        vec_d = self.inp(f"vec{l}", [128, 64])
        vec = P.sbuf("vec", [128, 64], F32, persist=True)
        vec_t = Tr()
        P.dma("sp", vec[:], vec_d[:, :], [], [vec_t])
        mod = P.sbuf("mod", [128, 48, 2], F32, persist=True)
        mod_t = Tr()
        wv = wada.rearrange("(k p) f -> p k f", p=128)
        wp = [(P.sbuf(f"wadap{i}", [128, 8, 512], F32), Tr()) for i in range(2)]
        ps = [(P.psum(f"modps{i}", [128, 512], F32), Tr(ps=True)) for i in range(2)]
        for i in range(12):
            wt, wt_t = wp[i % 2]
            P.dma("sp", wt[:], wv[:, :, i * 512:(i + 1) * 512], [], [wt_t])
            for fc in range(4):
                q = i * 4 + fc
                pt, pt_t = ps[q % 2]
                for k in range(8):
                    P.mm(pt[:, 0:2], wt[:, k, fc * 128:(fc + 1) * 128], self.scT[:, k, :], k == 0, k == 7,
                         [wt_t, self.scT_t], [pt_t])
                P.act(mod[:, q, :], pt[:, 0:2], AF.Identity, [pt_t, vec_t], [mod_t], bias=vec[:, q:q + 1])
        AB = P.sbuf("modAB", [128, 2, 8, 2], F32, persist=True)
        AB_t = Tr()
        for w_i, (sc0, g0) in enumerate(((8, 48), (32, 56))):
            for col in range(2):
                P.stt("dve", AB[:, w_i, :, col], mod[:, sc0:sc0 + 8, col], 1.0, vec[:, g0:g0 + 8], ALU.add, ALU.mult,
                      [mod_t, vec_t], [AB_t])
        self.mod, self.mod_t, self.AB, self.AB_t = mod, mod_t, AB, AB_t

    def mod_ap(self, which, c, col):
        return self.mod[:, which * 8 + c, col:col + 1]

    def norm_mod(self, hT, hT_t, uT, uT_t, which, f32cb=None, tile_cb=None, hget=None, tile_pre=None):
        P = self.P
        if hget is None:
            hget = lambda ti, c, s, w: hT[:, c, s:s + w]
        sq = [(P.sbuf(f"nm_sq{i}", [128, 8, 512], BF16), Tr()) for i in range(1)]
        ssp = [(P.psum(f"nm_ss{i}", [128, 512], F32), Tr(ps=True)) for i in range(2)]
        rs = [(P.sbuf(f"nm_rs{i}", [128, 512], F32), Tr()) for i in range(2)]
        tmp = [(P.sbuf(f"nm_tmp{i}", [128, 512], F32), Tr()) for i in range(3)]
        shq = 0 if which == 0 else 3
        for ti, (s, w) in enumerate(TILES):
            col = 0 if ti < 4 else 1
            if tile_pre is not None:
                tile_pre(ti)
            sqt, sqt_t = sq[0]
            pst, pst_t = ssp[ti % 2]
            rst, rst_t = rs[ti % 2]
            for c in range(8):
                P.act(sqt[:, c, :w], hget(ti, c, s, w), AF.Square, [hT_t[ti]], [sqt_t])
            for c in range(8):
                P.mm(pst[:, :w], self.onesb[:], sqt[:, c, :w], c == 0, c == 7, [self.onesb_t, sqt_t], [pst_t])
            P.act(rst[:, :w], pst[:, :w], AF.Sqrt, [pst_t], [rst_t], bias=EPS, scale=1.0 / D)
            P.op("dve", lambda e, a=rst[:, :w]: e.reciprocal(a, a), [rst_t], [rst_t])
            for c in range(8):
                tt_, tt_t = tmp[c % 3]
                P.stt("dve", tt_[:, :w], hget(ti, c, s, w), self.AB[:, which, c, col:col + 1], rst[:, :w],
                      ALU.mult, ALU.mult, [hT_t[ti], self.AB_t, rst_t], [tt_t])
                if f32cb is not None:
                    f32cb(ti, c, s, w, tt_, tt_t, self.mod_ap(shq, c, col))
                P.act(uT[:, c, s:s + w], tt_[:, :w], AF.Identity, [tt_t, self.mod_t], [uT_t[ti]],
                      bias=self.mod_ap(shq, c, col))
            if tile_cb is not None:
                tile_cb(ti, s, w)

    def moe(self, l, hT, hT_t, vT, vT_t, n_exp=NE, skip_ctx=False):
        P = self.P
        moe_mark = P.mark()
        wr_d = self.inp(f"wr{l}", [128, 8 * NE])
        br_d = self.inp(f"br{l}", [128, NE])
        bb_d = self.inp(f"bexp{l}", [128, NE * 24])
        wr = P.sbuf("wr", [128, 8, NE], F32)
        wr_t = Tr()
        P.dma("sp", wr[:].rearrange("p c e -> p (c e)"), wr_d[:, :], [], [wr_t])
        br = P.sbuf("br", [128, NE], F32)
        br_t = Tr()
        P.dma("sp", br[:], br_d[:, :], [], [br_t])
        bb = P.sbuf("bexp", [128, NE, 24], F32, mid=True)
        bb_t = Tr()
        P.dma("sp", bb[:].rearrange("p e c -> p (e c)"), bb_d[:, :], [], [bb_t])
        bsig = P.sbuf("bsig", [128, NE, 8], F32, mid=True)
        bsig_t = Tr()
        bl1 = P.sbuf("bl1", [128, NE, 8], F32, mid=True)
        bl1_t = Tr()
        P.ts("dve", bsig[:], bb[:, :, 0:8], 1.702, None, ALU.mult, None, [bb_t], [bsig_t])
        P.ts("dve", bl1[:], bb[:, :, 8:16], 1.0, None, ALU.add, None, [bb_t], [bl1_t])
        sel = P.sbuf("sel", [NE, NE, 128], BF16, mid=True)
        sel_t = Tr()
        P.memset("dve", sel[:], 0.0, [sel_t])
        P.op("pool", lambda e: e.affine_select(sel[:], sel[:], [[-1, NE], [0, 128]], ALU.not_equal, 1.0, base=0,
                                               channel_multiplier=1), [sel_t], [sel_t])
        vf = [(P.sbuf(f"vf{i}", [128, 8, 512], F32), Tr()) for i in range(1)]
        lg_ps = [(P.psum(f"lgps{i}", [128, NE], F32), Tr(ps=True)) for i in range(2)]
        gT = P.sbuf("gT", [NE, T], BF16, mid=True)
        gT_t = [Tr() for _ in TILES]
        gt_ps = [(P.psum(f"gtps{i}", [NE, 128], F32), Tr(ps=True)) for i in range(2)]
        sm = {}
        for nm, shp in (("lg", [128, NE]), ("mx", [128, 8]), ("nmx", [128, 1]), ("ex", [128, NE]),
                        ("mk", [128, NE]), ("sm", [128, 1]), ("g", [128, NE])):
            sm[nm] = [(P.sbuf(f"r_{nm}{i}", shp, F32), Tr()) for i in range(2)]
        cnt = [0]
        import os
        dbg = int(os.environ.get("MOE_DBG", "9"))

        def f32cb(ti, c, s, w, tt_, tt_t, bias_ap):
            vft, vft_t = vf[0]
            P.act(vft[:, c, :w], tt_[:, :w], AF.Identity, [tt_t, self.mod_t], [vft_t], bias=bias_ap)

        def tile_cb(ti, s, w):
            vft, vft_t = vf[0]
            for b0 in range(0, w, 128):
                bw = min(128, w - b0)
                i = cnt[0] % 2
                cnt[0] += 1
                lp, lp_t = lg_ps[i]
                for c in range(8):
                    P.mm(lp[:bw, :NE], vft[:, c, b0:b0 + bw], wr[:, c, :], c == 0, c == 7, [vft_t, wr_t], [lp_t])
                lg, lg_t = sm["lg"][i]
                mx, mx_t = sm["mx"][i]
                nmx, nmx_t = sm["nmx"][i]
                ex, ex_t = sm["ex"][i]
                mk, mk_t = sm["mk"][i]
                su, su_t = sm["sm"][i]
                g, g_t = sm["g"][i]
                P.tt("dve", lg[:bw, :], lp[:bw, :NE], br[:bw, :], ALU.add, [lp_t, br_t], [lg_t])
                if dbg == 3:
                    continue
                P.op("dve", lambda e, o=mx[:bw, :], a=lg[:bw, :]: e.max(out=o, in_=a), [lg_t], [mx_t])
                P.ts("dve", nmx[:bw, :], mx[:bw, 0:1], -1.0, None, ALU.mult, None, [mx_t], [nmx_t])
                P.act(ex[:bw, :], lg[:bw, :], AF.Exp, [lg_t, nmx_t], [ex_t], bias=nmx[:bw, 0:1], scale=1.0)
                P.ts("dve", mk[:bw, :], lg[:bw, :], mx[:bw, 3:4], None, ALU.is_ge, None, [lg_t, mx_t], [mk_t])
                P.tt("dve", ex[:bw, :], ex[:bw, :], mk[:bw, :], ALU.mult, [ex_t, mk_t], [ex_t])
                P.op("dve", lambda e, o=su[:bw, :], a=ex[:bw, :]: e.reduce_sum(o, a, AX.X), [ex_t], [su_t])
                P.op("dve", lambda e, a=su[:bw, :]: e.reciprocal(a, a), [su_t], [su_t])
                P.ts("dve", g[:bw, :], ex[:bw, :], su[:bw, 0:1], None, ALU.mult, None, [ex_t, su_t], [g_t])
                if dbg == 4:
                    continue
                gp, gp_t = gt_ps[i]
                P.tp(gp[:NE, :bw], g[:bw, :], self.identf[:bw, :bw], [g_t, self.identf_t], [gp_t])
                P.copy("act", gT[:, s + b0:s + b0 + bw], gp[:NE, :bw], [gp_t], [gT_t[ti]])

        if dbg == 1:
            P.flush()
            return
        self.norm_mod(hT, hT_t, vT, vT_t, 1, f32cb=f32cb if dbg >= 2 else None, tile_cb=tile_cb if dbg >= 3 else None)
        P.flush()
        if os.environ.get("MOE_STOP") == "1":
            return
        w1_d = self.inp(f"w1_{l}", [n_exp, D, 2 * D])
        w2_d = self.inp(f"w2_{l}", [n_exp, D, D])
        w1h = [P.sbuf(f"w1h{i}", [128, 8, D], BF16) for i in range(2)]
        w1h_t = [Tr(), Tr()]
        w2b = P.sbuf("w2b", [128, 8, D], BF16)
        w2_t = Tr()
        ym = [(P.sbuf(f"ym{i}", [128, 8, 512], BF16), Tr()) for i in range(2)]
        psg = [(P.psum(f"psg{i}", [128, 512], F32), Tr(ps=True)) for i in range(2)]
        psl = [(P.psum(f"psl{i}", [128, 512], F32), Tr(ps=True)) for i in range(2)]
        psy = [(P.psum(f"psy{i}", [128, 512], F32), Tr(ps=True)) for i in range(2)]
        psb = [(P.psum(f"psb{i}", [128, 512], F32), Tr(ps=True)) for i in range(1)]
        gB = [(P.sbuf(f"gB{i}", [128, 512], F32), Tr()) for i in range(2)]
        xg = [(P.sbuf(f"xg{i}", [128, 512], F32), Tr()) for i in range(2)]
        sg = [(P.sbuf(f"sg{i}", [128, 512], F32), Tr()) for i in range(2)]
        xa = [(P.sbuf(f"xa{i}", [128, 512], F32), Tr()) for i in range(2)]
        tm = [(P.sbuf(f"tm{i}", [128, 512], F32), Tr()) for i in range(2)]
        it = 0
        jj = 0
        cc = 0
        for e_ in range(n_exp):
            w1v = w1_d[e_].rearrange("(k p) f -> p k f", p=128)
            for hf in range(2):
                P.dma("pool", w1h[hf][:, :, 0:512], w1v[:, :, hf * 512:(hf + 1) * 512], [], [w1h_t[hf]])
                P.dma("pool", w1h[hf][:, :, 512:1024], w1v[:, :, D + hf * 512:D + (hf + 1) * 512], [], [w1h_t[hf]])
            P.dma("pool", w2b[:], w2_d[e_].rearrange("(k p) f -> p k f", p=128), [], [w2_t])
            xd = int(os.environ.get("EXP_DBG", "9"))
            for ti in ((0, 1, 2, 3) if skip_ctx else (4, 0, 1, 2, 3)):
                s, w = TILES[ti]
                if xd == 1:
                    break
                ymt, ymt_t = ym[it % 2]
                gBt, gBt_t = gB[it % 2]
                it += 1
                pb, pb_t = psb[0]
                P.mm(pb[:, :w], sel[:, e_, :], gT[:, s:s + w], True, True, [sel_t, gT_t[ti]], [pb_t])
                P.copy("act", gBt[:, :w], pb[:, :w], [pb_t], [gBt_t])
                if xd == 2:
                    continue
                for j in range(8):
                    pg, pg_t = psg[jj % 2]
                    pl, pl_t = psl[jj % 2]
                    xgt, xgt_t = xg[jj % 2]
                    sgt, sgt_t = sg[jj % 2]
                    xat, xat_t = xa[jj % 2]
                    jj += 1
                    wh, wh_t = w1h[j // 4], w1h_t[j // 4]
                    jc = (j % 4) * 128
                    for k in range(8):
                        P.mm(pg[:, :w], wh[:, k, jc:jc + 128], vT[:, k, s:s + w], k == 0, k == 7,
                             [wh_t, vT_t[ti]], [pg_t])
                    for k in range(8):
                        P.mm(pl[:, :w], wh[:, k, 512 + jc:512 + jc + 128], vT[:, k, s:s + w], k == 0, k == 7,
                             [wh_t, vT_t[ti]], [pl_t])
                    if xd == 3:
                        continue
                    P.act(sgt[:, :w], pg[:, :w], AF.Sigmoid, [pg_t, bsig_t], [sgt_t], bias=bsig[:, e_, j:j + 1], scale=1.702)
                    P.ts("dve", xgt[:, :w], pg[:, :w], bb[:, e_, j:j + 1], self.cst[:, 0:1], ALU.add, ALU.min, [pg_t, bb_t, self.cst_t], [xgt_t])
                    P.stt("dve", xgt[:, :w], sgt[:, :w], 0.9999933, xgt[:, :w], ALU.min, ALU.mult, [sgt_t, xgt_t], [xgt_t])
                    P.ts("dve", xat[:, :w], pl[:, :w], bl1[:, e_, j:j + 1], self.cst[:, 1:2], ALU.add, ALU.max, [pl_t, bl1_t, self.cst_t], [xat_t])
                    P.stt("dve", ymt[:, j, :w], xat[:, :w], 8.0, xgt[:, :w], ALU.min, ALU.mult, [xat_t, xgt_t], [ymt_t])
                col = 0 if ti < 4 else 1
                if xd <= 4:
                    continue
                for c in range(8):
                    py, py_t = psy[cc % 2]
                    tmt, tmt_t = tm[cc % 2]
                    cc += 1
                    for j in range(8):
                        P.mm(py[:, :w], w2b[:, j, c * 128:(c + 1) * 128], ymt[:, j, :w], j == 0, j == 7,
                             [w2_t, ymt_t], [py_t])
                    P.stt("dve", tmt[:, :w], py[:, :w], bb[:, e_, 16 + c:17 + c], gBt[:, :w], ALU.add, ALU.mult,
                          [py_t, bb_t, gBt_t], [tmt_t])
                    P.stt("dve", hT[:, c, s:s + w], tmt[:, :w], self.mod_ap(5, c, col), hT[:, c, s:s + w],
                          ALU.mult, ALU.add, [tmt_t, self.mod_t, hT_t[ti]], [hT_t[ti]])
        P.flush()
        P.release(moe_mark)

    def ab_scratch(self):
        nc = self.nc
        if hasattr(self, "KA"):
            return
        self.KA = nc.dram_tensor("KA", [8, 65, NKEY], BF16).ap()
        self.VA = nc.dram_tensor("VA", [4, NKEY, 128], BF16).ap()
        self.KB = nc.dram_tensor("KB", [4, 128, NKEY], BF16).ap()
        self.VB = nc.dram_tensor("VB", [4, NKEY, 128], BF16).ap()
        self.KR = nc.dram_tensor("KR", [65, NKEY], BF16).ap()
        self.kv_t = [Tr() for _ in range(4)]
        self.QA = nc.dram_tensor("QA", [4, 8, 65, T], BF16).ap()
        self.QBn = nc.dram_tensor("QBn", [4, 4, 128, T], BF16).ap()
        self.QBr = nc.dram_tensor("QBr", [4, 4, 65, T], BF16).ap()
        self.q_t = [Tr() for _ in range(4)]

    def ab_weights(self, j):
        P = self.P
        rot_d = self.inp("rotPT", [64, 64])
        self.rotPT = P.sbuf("rotPT", [64, 64], BF16, mid=True)
        self.rot_t = Tr()
        P.dma("pool", self.rotPT[:], rot_d[:, :], [], [self.rot_t])
        sv_d = self.inp(f"abvec{j}", [128, 8])
        self.abv = P.sbuf("abv", [128, 8], F32, mid=True)
        self.abv_t = Tr()
        P.dma("sp", self.abv[:], sv_d[:, :], [], [self.abv_t])
        self.win_mark = P.mark()
        win_d = self.inp(f"win{j}", [D, 1984])
        self.win = P.sbuf("win", [128, 8, 1984], BF16, mid=True)
        self.win_t = Tr()
        P.dma("pool", self.win[:], win_d.rearrange("(k p) f -> p k f", p=128), [], [self.win_t])

    def rope_tables(self, g):
        P = self.P
        cos_d = self.inp("rope_cos", [4, 64, T])
        sin_d = self.inp("rope_sin", [4, 64, T])
        self.COS = P.sbuf("COS", [64, T], F32)
        self.SIN = P.sbuf("SIN", [64, T], F32)
        self.cs_t = Tr()
        P.dma("sp", self.COS[:], cos_d[g], [], [self.cs_t])
        P.dma("sp", self.SIN[:], sin_d[g], [], [self.cs_t])

    def inp(self, name, shape, dtype=F32):
        if name in self.ins:
            return self.ins[name]
        t = self.nc.dram_tensor(name, list(shape), dtype, kind="ExternalInput").ap()
        self.ins[name] = t
        return t

    def rope64(self, ps, ps_t, s, w, dst, dst_t, tmp, pp, pp_t, ssq=None):
        P = self.P
        xb, xb_t = tmp["xb"].next()
        t1, t1_t = tmp["t1"].next()
        t2, t2_t = tmp["t2"].next()
        P.copy("act", xb[:64, :w], ps, [ps_t], [xb_t])
        P.mm(pp[:64, :w], self.rotPT[:, :], xb[:64, :w], True, True, [self.rot_t, xb_t], [pp_t])
        P.tt("dve", t1[:64, :w], ps, self.COS[:, s:s + w], ALU.mult, [ps_t, self.cs_t], [t1_t])
        P.tt("dve", t2[:64, :w], pp[:64, :w], self.SIN[:, s:s + w], ALU.mult, [pp_t, self.cs_t], [t2_t])
        P.tt("pool", dst, t1[:64, :w], t2[:64, :w], ALU.add, [t1_t, t2_t], [dst_t])
        if ssq is not None:
            sq, sq_t = tmp["sq"].next()
            sp, sp_t = ssq
            P.act(sq[:64, :w], dst, AF.Square, [dst_t], [sq_t])
            P.mm(sp[:, :w], self.onesb[:64, :], sq[:64, :w], True, True, [self.onesb_t, sq_t], [sp_t])

    def key_cols(self, g, ti):
        s, w = TILES[ti]
        if ti < 4:
            return CTX + g * NLAT + s
        return g * NCTX

    def ab_pre(self, j, g, uT, uT_t):
        P = self.P
        self.ab_scratch()
        wukv_d = self.inp(f"wukv{j}", [128, 1024])
        wukv = P.sbuf("wukv", [128, 1024], BF16)
        wukv_t = Tr()
        P.dma("pool", wukv[:], wukv_d[:, :], [], [wukv_t])
        self.rope_tables(g)
        mk = lambda nm, shp, dt, n: Rot([(P.sbuf(f"{nm}{i}", shp, dt), Tr()) for i in range(n)])
        tmp = {"xb": mk("xb", [64, 512], BF16, 2), "t1": mk("t1", [64, 512], F32, 2), "t2": mk("t2", [64, 512], F32, 2),
               "sq": mk("sq", [128, 512], BF16, 2)}
        kst = mk("kst", [65, 512], BF16, 3)
        vst = mk("vst", [128, 512], BF16, 2)
        kbst = mk("kbst", [128, 512], BF16, 2)
        ckn = mk("ckn", [128, 512], BF16, 2)
        rsd = mk("rsd", [128, 512], F32, 2)
        mxt = mk("mxt", [128, 1], F32, 2)
        psA = Rot([(P.psum(), Tr(ps=True)) for _ in range(2)])
        psP = Rot([(P.psum(), Tr(ps=True)) for _ in range(1)])
        psS = Rot([(P.psum(), Tr(ps=True)) for _ in range(2)])
        psV = Rot([(P.psum(), Tr(ps=True)) for _ in range(1)])
        win, win_t = self.win, self.win_t
        kvt = self.kv_t[g]
        for ti, (s, w) in enumerate(TILES):
            kc0 = self.key_cols(g, ti)
            for ph in range(8):
                ps, ps_t = psA.next()
                for k in range(8):
                    P.mm(ps[:64, :w], win[:, k, 512 + ph * 64:512 + (ph + 1) * 64], uT[:, k, s:s + w], k == 0, k == 7,
                         [win_t, uT_t[ti]], [ps_t])
                kt, kt_t = kst.next()
                pp, pp_t = psP.next()
                sp = psS.next()
                self.rope64(ps[:64, :w], ps_t, s, w, kt[0:64, :w], kt_t, tmp, pp, pp_t, ssq=sp)
                P.memset("pool", kt[64:65, :w], 1.0, [kt_t])
                m, m_t = mxt.next()
                P.op("dve", lambda e, o=m[:, :], a=sp[0][:, :w]: e.reduce_max(o, a, AX.X), [sp[1]], [m_t])
                P.tt("dve", self.kmaxA[:, :], self.kmaxA[:, :], m[:, :], ALU.max, [self.kmaxA_t, m_t], [self.kmaxA_t])
                P.dma("sp", self.KA[ph, :, kc0:kc0 + w], kt[:, :w], [kt_t], [kvt])
            ps, ps_t = psA.next()
            for k in range(8):
                P.mm(ps[:64, :w], win[:, k, 1920:1984], uT[:, k, s:s + w], k == 0, k == 7, [win_t, uT_t[ti]], [ps_t])
            kt, kt_t = kst.next()
            pp, pp_t = psP.next()
            spr = psS.next()
            self.rope64(ps[:64, :w], ps_t, s, w, kt[0:64, :w], kt_t, tmp, pp, pp_t, ssq=spr)
            P.memset("pool", kt[64:65, :w], 1.0, [kt_t])
            m, m_t = mxt.next()
            P.op("dve", lambda e, o=m[:, :], a=spr[0][:, :w]: e.reduce_max(o, a, AX.X), [spr[1]], [m_t])
            P.tt("dve", self.kmaxB[:, 0:1], self.kmaxB[:, 0:1], m[:, :], ALU.max, [self.kmaxB_t, m_t], [self.kmaxB_t])
            P.dma("sp", self.KR[:, kc0:kc0 + w], kt[:, :w], [kt_t], [kvt])
            ps, ps_t = psA.next()
            for k in range(8):
                P.mm(ps[:, :w], win[:, k, 1792:1920], uT[:, k, s:s + w], k == 0, k == 7, [win_t, uT_t[ti]], [ps_t])
            sq, sq_t = tmp["sq"].next()
            P.act(sq[:, :w], ps[:, :w], AF.Square, [ps_t], [sq_t])
            sp, sp_t = psS.next()
            P.mm(sp[:, :w], self.onesb[:, :], sq[:, :w], True, True, [self.onesb_t, sq_t], [sp_t])
            rs, rs_t = rsd.next()
            P.act(rs[:, :w], sp[:, :w], AF.Sqrt, [sp_t], [rs_t], bias=EPS, scale=1.0 / 128)
            P.op("dve", lambda e, a=rs[:, :w]: e.reciprocal(a, a), [rs_t], [rs_t])
            ck, ck_t = ckn.next()
            P.stt("dve", ck[:, :w], ps[:, :w], self.abv[:, 2:3], rs[:, :w], ALU.mult, ALU.mult, [ps_t, self.abv_t, rs_t], [ck_t])
            for h in range(4):
                pk, pk_t = psA.next()
                P.mm(pk[:, :w], wukv[:, h * 256:h * 256 + 128], ck[:, :w], True, True, [wukv_t, ck_t], [pk_t])
                kb, kb_t = kbst.next()
                P.copy("act", kb[:, :w], pk[:, :w], [pk_t], [kb_t])
                sq, sq_t = tmp["sq"].next()
                P.act(sq[:, :w], kb[:, :w], AF.Square, [kb_t], [sq_t])
                sp, sp_t = psS.next()
                P.mm(sp[:, :w], self.onesb[:, :], sq[:, :w], True, True, [self.onesb_t, sq_t], [sp_t])
                m, m_t = mxt.next()
                P.op("dve", lambda e, o=m[:, :], a=sp[:, :w]: e.reduce_max(o, a, AX.X), [sp_t], [m_t])
                P.tt("dve", self.kmaxB[:, 1:2], self.kmaxB[:, 1:2], m[:, :], ALU.max, [self.kmaxB_t, m_t], [self.kmaxB_t])
                P.dma("sp", self.KB[h, :, kc0:kc0 + w], kb[:, :w], [kb_t], [kvt])
            for b0 in range(0, w, 128):
                bw = min(128, w - b0)
                pv, pv_t = psV.next()
                for k in range(8):
                    P.mm(pv[:bw, :], uT[:, k, s + b0:s + b0 + bw], win[:, k, 1024:1536], k == 0, k == 7,
                         [uT_t[ti], win_t], [pv_t])
                vt, vt_t = vst.next()
                P.copy("act", vt[:bw, :], pv[:bw, :], [pv_t], [vt_t])
                P.dma("sp", self.VA[:, kc0 + b0:kc0 + b0 + bw, :].rearrange("h k d -> k h d"),
                      vt[:bw, :].rearrange("k (h d) -> k h d", h=4), [vt_t], [kvt])
                pv, pv_t = psV.next()
                for h in range(4):
                    P.mm(pv[:bw, h * 128:(h + 1) * 128], ck[:, b0:b0 + bw], wukv[:, h * 256 + 128:h * 256 + 256], True, True,
                         [ck_t, wukv_t], [pv_t])
                vt, vt_t = vst.next()
                P.copy("dve", vt[:bw, :], pv[:bw, :], [pv_t], [vt_t])
                P.dma("sp", self.VB[:, kc0 + b0:kc0 + b0 + bw, :].rearrange("h k d -> k h d"),
                      vt[:bw, :].rearrange("k (h d) -> k h d", h=4), [vt_t], [kvt])
        self.ab_q(j, g, uT, uT_t, {"tmp": tmp, "psA": psA, "psP": psP, "psS": psS, "kst": kst, "rsd": rsd})

    def ab_layer_init(self):
        P = self.P
        P.memset("dve", self.kmaxA[:, :], 0.0, [self.kmaxA_t])
        P.memset("dve", self.kmaxB[:, :], 0.0, [self.kmaxB_t])

    def ab_q(self, j, g, uT, uT_t, shared):
        P = self.P
        tmp, psA, psP, psS, kst, rsd = (shared[k] for k in ("tmp", "psA", "psP", "psS", "kst", "rsd"))
        win, win_t = self.win, self.win_t
        wuq_d = self.inp(f"wuq{j}", [256, 768])
        wuq = P.sbuf("wuq", [128, 2, 768], BF16)
        wuq_t = Tr()
        P.dma("pool", wuq[:], wuq_d.rearrange("(c p) f -> p c f", p=128), [], [wuq_t])
        mk = lambda nm, shp, dt, n: Rot([(P.sbuf(f"{nm}{i}", shp, dt), Tr()) for i in range(n)])
        cqf = mk("cqf", [128, 2, 512], F32, 1)
        cqn = mk("cqn", [128, 2, 512], BF16, 2)
        qnb = mk("qnb", [128, 512], BF16, 2)
        axr = mk("axr", [65, 512], F32, 2)
        qt = self.q_t[g]
        for ti, (s, w) in enumerate(TILES):
            for ph in range(8):
                ps, ps_t = psA.next()
                for k in range(8):
                    P.mm(ps[:64, :w], win[:, k, ph * 64:(ph + 1) * 64], uT[:, k, s:s + w], k == 0, k == 7,
                         [win_t, uT_t[ti]], [ps_t])
                kt, kt_t = kst.next()
                pp, pp_t = psP.next()
                sp = psS.next()
                self.rope64(ps[:64, :w], ps_t, s, w, kt[0:64, :w], kt_t, tmp, pp, pp_t, ssq=sp)
                ax, ax_t = axr.next()
                P.act(ax[64:65, :w], sp[0][64:65, :w], AF.Sqrt, [sp[1]], [ax_t])
                P.copy("dve", kt[64:65, :w], ax[64:65, :w], [ax_t], [kt_t])
                P.dma("sp", self.QA[g, ph, :, s:s + w], kt[:, :w], [kt_t], [qt])
            cf, cf_t = cqf.next()
            sp, sp_t = psS.next()
            for cc in range(2):
                ps, ps_t = psA.next()
                for k in range(8):
                    P.mm(ps[:, :w], win[:, k, 1536 + cc * 128:1536 + (cc + 1) * 128], uT[:, k, s:s + w], k == 0, k == 7,
                         [win_t, uT_t[ti]], [ps_t])
                P.copy("dve", cf[:, cc, :w], ps[:, :w], [ps_t], [cf_t])
                sq, sq_t = tmp["sq"].next()
                P.act(sq[:, :w], cf[:, cc, :w], AF.Square, [cf_t], [sq_t])
                P.mm(sp[:, :w], self.onesb[:, :], sq[:, :w], cc == 0, cc == 1, [self.onesb_t, sq_t], [sp_t])
            rs, rs_t = rsd.next()
            P.act(rs[:, :w], sp[:, :w], AF.Sqrt, [sp_t], [rs_t], bias=EPS, scale=1.0 / 256)
            P.op("dve", lambda e, a=rs[:, :w]: e.reciprocal(a, a), [rs_t], [rs_t])
            cn, cn_t = cqn.next()
            for cc in range(2):
                P.stt("dve", cn[:, cc, :w], cf[:, cc, :w], self.abv[:, cc:cc + 1], rs[:, :w], ALU.mult, ALU.mult,
                      [cf_t, self.abv_t, rs_t], [cn_t])
            for h in range(4):
                ps, ps_t = psA.next()
                for cc in range(2):
                    P.mm(ps[:, :w], wuq[:, cc, h * 192:h * 192 + 128], cn[:, cc, :w], cc == 0, cc == 1, [wuq_t, cn_t], [ps_t])
                qb, qb_t = qnb.next()
                P.copy("act", qb[:, :w], ps[:, :w], [ps_t], [qb_t])
                P.dma("sp", self.QBn[g, h, :, s:s + w], qb[:, :w], [qb_t], [qt])
                sq, sq_t = tmp["sq"].next()
                P.act(sq[:, :w], qb[:, :w], AF.Square, [qb_t], [sq_t])
                spn, spn_t = psS.next()
                P.mm(spn[:, :w], self.onesb[:, :], sq[:, :w], True, True, [self.onesb_t, sq_t], [spn_t])
                ps, ps_t = psA.next()
                for cc in range(2):
                    P.mm(ps[:64, :w], wuq[:, cc, h * 192 + 128:h * 192 + 192], cn[:, cc, :w], cc == 0, cc == 1,
                         [wuq_t, cn_t], [ps_t])
                kt, kt_t = kst.next()
                pp, pp_t = psP.next()
                sp2 = psS.next()
                self.rope64(ps[:64, :w], ps_t, s, w, kt[0:64, :w], kt_t, tmp, pp, pp_t, ssq=sp2)
                ax, ax_t = axr.next()
                P.copy("dve", ax[64:65, :w], spn[64:65, :w], [spn_t], [ax_t])
                P.tt("dve", ax[64:65, :w], ax[64:65, :w], sp2[0][64:65, :w], ALU.add, [ax_t, sp2[1]], [ax_t])
                P.act(ax[64:65, :w], ax[64:65, :w], AF.Sqrt, [ax_t], [ax_t])
                P.copy("dve", kt[64:65, :w], ax[64:65, :w], [ax_t], [kt_t])
                P.dma("sp", self.QBr[g, h, :, s:s + w], kt[:, :w], [kt_t], [qt])

    def ab_attn(self, j, g, oT, oT_t):
        P = self.P
        lam_init = 0.8 - 0.6 * math.exp(-0.3 * (2 * j))
        oml = 1.0 - lam_init
        A_SCALE = 64 ** -0.5
        B_SCALE = 192 ** -0.5
        mk = lambda nm, shp, dt, n: Rot([(P.sbuf(f"{nm}{i}", shp, dt), Tr()) for i in range(n)])
        bank = [(P.psum(), Tr(ps=True)) for _ in range(8)]
        psS = Rot(bank[0:4])
        psO = Rot(bank[4:6])
        psD = Rot(bank[6:8])
        psX = psS
        dl_d = self.inp(f"dlam{j}", [128, 256])
        dl = P.sbuf("dl", [128, 256], F32)
        dl_t = Tr()
        P.dma("sp", dl[:], dl_d[:, :], [], [dl_t])
        lam = P.sbuf("lam", [128, 4], F32)
        lam_t = Tr()
        pr = P.sbuf("dlp", [128, 128], F32)
        pr_t = Tr()
        P.tt("dve", pr[:, 0:64], dl[:, 0:64], dl[:, 64:128], ALU.mult, [dl_t], [pr_t])
        P.tt("dve", pr[:, 64:128], dl[:, 128:192], dl[:, 192:256], ALU.mult, [dl_t], [pr_t])
        P.op("dve", lambda e: e.reduce_sum(lam[:, 0:1], pr[:, 0:64], AX.X), [pr_t], [lam_t])
        P.op("dve", lambda e: e.reduce_sum(lam[:, 1:2], pr[:, 64:128], AX.X), [pr_t], [lam_t])
        P.act(lam[:, 0:2], lam[:, 0:2], AF.Exp, [lam_t], [lam_t])
        P.tt("dve", lam[:, 2:3], lam[:, 0:1], lam[:, 1:2], ALU.subtract, [lam_t], [lam_t])
        P.ts("dve", lam[:, 3:4], lam[:, 2:3], lam_init, -1.0, ALU.add, ALU.mult, [lam_t], [lam_t])
        nk = P.sbuf("nk", [128, 2], F32)
        nk_t = Tr()
        P.act(nk[:, 0:1], self.kmaxA[:, 0:1], AF.Sqrt, [self.kmaxA_t], [nk_t])
        kb2 = P.sbuf("kb2", [128, 1], F32)
        kb2_t = Tr()
        P.tt("dve", kb2[:, :], self.kmaxB[:, 0:1], self.kmaxB[:, 1:2], ALU.add, [self.kmaxB_t], [kb2_t])
        P.act(nk[:, 1:2], kb2[:, :], AF.Sqrt, [kb2_t], [nk_t])
        P.ts("dve", nk[:, :], nk[:, :], -1.0, None, ALU.mult, None, [nk_t], [nk_t])
        kbuf = mk("kbuf", [128, NKEY], BF16, 3)
        vbuf = mk("vbuf", [128, NKEY // 128, 128], BF16, 2)
        qaug = mk("qaug", [65, T], BF16, 3)
        qn = mk("qn", [128, T], BF16, 2)
        pT = mk("pT", [128, 512], BF16, 4)
        of = mk("of", [128, 512], F32, 3)
        rc = mk("rc", [128, 512], F32, 2)
        sqb = mk("sqb", [128, 512], BF16, 2)
        kvall = self.kv_t
        qt = self.q_t[g]

        def load_q(src, fam):
            qa, qa_t = qaug.next()
            P.dma("sp", qa[0:65, :], src, [qt], [qa_t])
            P.ts("dve", qa[64:65, :], qa[64:65, :], nk[64:65, fam:fam + 1], None, ALU.mult, None, [qa_t, nk_t], [qa_t])
            return qa, qa_t

        def attend(kparts, q_parts, v, v_t, scale, ti):
            s, w = TILES[ti]
            nkc = NKEY // 128 if ti < 4 else CTX // 128
            po, po_t = psO.next()
            pd, pd_t = psD.next()

            def scores(kc):
                pss, pss_t = psS.next()
                for i_, ((kf, k_t), (qa, q_t)) in enumerate(zip(kparts, q_parts)):
                    P.mm(pss[:, :w], kf(kc), qa, i_ == 0, i_ == len(kparts) - 1, [k_t, q_t], [pss_t])
                return pss, pss_t

            pend = [scores(kc) for kc in range(min(3, nkc))]
            for kc in range(nkc):
                pss, pss_t = pend.pop(0)
                if kc + 3 < nkc:
                    pend.append(scores(kc + 3))
                pt, pt_t = pT.next()
                P.act(pt[:, :w], pss[:, :w], AF.Exp, [pss_t], [pt_t], scale=scale)
                P.mm(po[:, :w], v[:, kc, :], pt[:, :w], kc == 0, kc == nkc - 1, [v_t, pt_t], [po_t])
                P.mm(pd[:, :w], self.onesb[:, :], pt[:, :w], kc == 0, kc == nkc - 1, [self.onesb_t, pt_t], [pd_t])
            return (po, po_t), (pd, pd_t)

        def normalize(po, pd, ti, dst, dst_t):
            s, w = TILES[ti]
            r, r_t = rc.next()
            P.op("dve", lambda e, o=r[:, :w], a=pd[0][:, :w]: e.reciprocal(o, a), [pd[1]], [r_t])
            P.tt("dve", dst, po[0][:, :w], r[:, :w], ALU.mult, [po[1], r_t], [dst_t])

        for h in range(4):
            v, v_t = vbuf.next()
            P.dma("sp", v[:], self.VA[h].rearrange("(c p) d -> p c d", p=128), kvall, [v_t])
            kk = []
            qq = []
            for mp in range(2):
                ph = 2 * h + mp
                kb, kb_t = kbuf.next()
                P.dma("sp", kb[0:65, :], self.KA[ph], kvall, [kb_t])
                kk.append((kb, kb_t))
                qq.append(load_q(self.QA[g, ph], 0))
            for ti, (s, w) in enumerate(TILES):
                o12 = []
                for mp in range(2):
                    kb, kb_t = kk[mp]
                    qa, qa_t = qq[mp]
                    po, pd = attend([(lambda kc, kb=kb: kb[0:65, kc * 128:(kc + 1) * 128], kb_t)],
                                    [(qa[0:65, s:s + w], qa_t)], v, v_t, A_SCALE, ti)
                    o, o_t = of.next()
                    normalize(po, pd, ti, o[:, :w], o_t)
                    o12.append((o, o_t))
                (o1, o1_t), (o2, o2_t) = o12
                P.stt("dve", o1[:, :w], o2[:, :w], lam[:, 3:4], o1[:, :w], ALU.mult, ALU.add, [o2_t, lam_t, o1_t], [o1_t])
                sq, sq_t = sqb.next()
                P.act(sq[:, :w], o1[:, :w], AF.Square, [o1_t], [sq_t])
                sp, sp_t = psX.next()
                P.mm(sp[:, :w], self.onesb[:, :], sq[:, :w], True, True, [self.onesb_t, sq_t], [sp_t])
                r, r_t = rc.next()
                P.act(r[:, :w], sp[:, :w], AF.Sqrt, [sp_t], [r_t], bias=EPS / (oml * oml), scale=1.0 / (128 * oml * oml))
                P.op("dve", lambda e, a=r[:, :w]: e.reciprocal(a, a), [r_t], [r_t])
                P.stt("dve", oT[:, h, s:s + w], o1[:, :w], self.abv[:, 3:4], r[:, :w], ALU.mult, ALU.mult,
                      [o1_t, self.abv_t, r_t], [oT_t[ti]])
        kr, kr_t = kbuf.next()
        P.dma("sp", kr[0:65, :], self.KR[:, :], kvall, [kr_t])
        for h in range(4):
            v, v_t = vbuf.next()
            P.dma("sp", v[:], self.VB[h].rearrange("(c p) d -> p c d", p=128), kvall, [v_t])
            kb, kb_t = kbuf.next()
            if kb is kr:
                kb, kb_t = kbuf.next()
            P.dma("sp", kb[:, :], self.KB[h], kvall, [kb_t])
            qa, qa_t = load_q(self.QBr[g, h], 1)
            qnt, qnt_t = qn.next()
            P.dma("sp", qnt[:, :], self.QBn[g, h], [qt], [qnt_t])
            for ti, (s, w) in enumerate(TILES):
                po, pd = attend([(lambda kc, kb=kb: kb[:, kc * 128:(kc + 1) * 128], kb_t),
                                 (lambda kc, kr=kr: kr[0:65, kc * 128:(kc + 1) * 128], kr_t)],
                                [(qnt[:, s:s + w], qnt_t), (qa[0:65, s:s + w], qa_t)], v, v_t, B_SCALE, ti)
                normalize(po, pd, ti, oT[:, 4 + h, s:s + w], oT_t[ti])

    def out_proj(self, wname, hT, hT_t, oT, oT_t):
        P = self.P
        wo_d = self.inp(wname, [D, D])
        wo = P.sbuf("wo", [128, 8, D], BF16)
        wo_t = Tr()
        P.dma("pool", wo[:], wo_d.rearrange("(k p) f -> p k f", p=128), [], [wo_t])
        psY = Rot([(P.psum(), Tr(ps=True)) for _ in range(3)])
        for ti, (s, w) in enumerate(TILES):
            col = 0 if ti < 4 else 1
            for c in range(8):
                py, py_t = psY.next()
                for k in range(8):
                    P.mm(py[:, :w], wo[:, k, c * 128:(c + 1) * 128], oT[:, k, s:s + w], k == 0, k == 7, [wo_t, oT_t[ti]], [py_t])
                P.stt("dve", hT[:, c, s:s + w], py[:, :w], self.mod_ap(2, c, col), hT[:, c, s:s + w], ALU.mult, ALU.add,
                      [py_t, self.mod_t, hT_t[ti]], [hT_t[ti]])

    def h_scratch(self):
        if not hasattr(self, "H"):
            self.H = self.nc.dram_tensor("Hs", [4, D, T], F32).ap()
            self.H_t = [Tr() for _ in range(4)]
            self.xT = self.inp("xT", [4, D, T])

    def load_h(self, src, src_t, hT, hT_t):
        hv = src.rearrange("(c p) t -> p c t", p=128)
        for ti, (s, w) in enumerate(TILES):
            self.P.dma("sp", hT[:, :, s:s + w], hv[:, :, s:s + w], src_t, [hT_t[ti]])

    def store_h(self, dst, dst_t, hT, hT_t):
        hv = dst.rearrange("(c p) t -> p c t", p=128)
        for ti, (s, w) in enumerate(TILES):
            self.P.dma("sp", hv[:, :, s:s + w], hT[:, :, s:s + w], [hT_t[ti]], dst_t)

    def norm_stream(self, src, src_t, uT, uT_t, which):
        P = self.P
        hb = [P.sbuf(f"hstr{i}", [128, 8, 512], F32) for i in range(2)]
        hb_t = [Tr(), Tr()]
        hv = src.rearrange("(c p) t -> p c t", p=128)

        def issue(ti):
            s, w = TILES[ti]
            P.dma("sp", hb[ti % 2][:, :, :w], hv[:, :, s:s + w], src_t, [hb_t[ti % 2]])

        def tile_pre(ti):
            if ti == 0:
                issue(0)
            if ti + 1 < len(TILES):
                issue(ti + 1)

        self.norm_mod(None, [hb_t[ti % 2] for ti in range(len(TILES))], uT, uT_t, which,
                      hget=lambda ti, c, s, w: hb[ti % 2][:, c, :w], tile_pre=tile_pre)

    def h_src(self, l, g):
        self.h_scratch()
        if l == 0:
            return self.xT[g], []
        return self.H[g], [self.H_t[g]]

    def ab_layer(self, l, groups=(0, 1, 2, 3), attn_groups=(0, 1, 2, 3), do_moe=True, n_exp=NE):
        P = self.P
        j = l // 2
        self.h_scratch()
        self.mods(l)
        self.ab_layer_init()
        P.flush()
        m0 = P.mark()
        self.ab_weights(j)
        for g in groups:
            uT = P.sbuf("uT", [128, 8, T], BF16)
            uT_t = [Tr() for _ in TILES]
            src, src_t = self.h_src(l, g)
            self.norm_stream(src, src_t, uT, uT_t, 0)
            self.ab_pre(j, g, uT, uT_t)
            P.flush()
        P.release(self.win_mark)
        for g in attn_groups:
            m1 = P.mark()
            oT = P.sbuf("oT", [128, 8, T], BF16, mid=True)
            oT_t = [Tr() for _ in TILES]
            self.ab_attn(j, g, oT, oT_t)
            P.flush()
            hT = P.sbuf("hT", [128, 8, T], F32, mid=True)
            hT_t = [Tr() for _ in TILES]
            src, src_t = self.h_src(l, g)
            self.load_h(src, src_t, hT, hT_t)
            self.out_proj(f"woutab{j}", hT, hT_t, oT, oT_t)
            P.flush()
            if do_moe:
                vT_t = [Tr() for _ in TILES]
                self.moe(l, hT, hT_t, oT, vT_t, n_exp=n_exp)
            self.store_h(self.H[g], [self.H_t[g]], hT, hT_t)
            P.flush()
            P.release(m1)
        P.release(m0)


def _pm(v):
    return np.ascontiguousarray(np.asarray(v, np.float32).reshape(-1, 128).T)


def host_common(c_b, c_ctx):
    rotP = np.zeros((64, 64), np.float32)
    for i in list(range(0, 16)) + list(range(32, 48)):
        rotP[i, i + 16] = -1.0
        rotP[i + 16, i] = 1.0
    inv = (10000.0 ** (-np.arange(16, dtype=np.float32) / 16)).astype(np.float32)
    cos = np.ones((4, 64, T), np.float32)
    sin = np.zeros((4, 64, T), np.float32)
    for g in range(4):
        t = g * NLAT + np.arange(NLAT)
        row = (t // 64).astype(np.float32)
        col = (t % 64).astype(np.float32)
        ar = (row[None, :] * inv[:, None]).astype(np.float32)
        ac = (col[None, :] * inv[:, None]).astype(np.float32)
        cos[g, 0:16, :NLAT] = np.cos(ar)
        cos[g, 16:32, :NLAT] = np.cos(ar)
        cos[g, 32:48, :NLAT] = np.cos(ac)
        cos[g, 48:64, :NLAT] = np.cos(ac)
        sin[g, 0:16, :NLAT] = np.sin(ar)
        sin[g, 16:32, :NLAT] = np.sin(ar)
        sin[g, 32:48, :NLAT] = np.sin(ac)
        sin[g, 48:64, :NLAT] = np.sin(ac)
    return {
        "ident": np.eye(128, dtype=np.float32),
        "cT": np.ascontiguousarray(np.stack([_pm(c_b), _pm(c_ctx)], -1).reshape(128, 16)),
        "rotPT": np.ascontiguousarray(rotP.T),
        "rope_cos": cos,
        "rope_sin": sin,
    }


def host_layer_vec(l, wada, bada, gmix, gffn):
    return {f"wada{l}": np.ascontiguousarray(wada, np.float32),
            f"vec{l}": np.ascontiguousarray(np.concatenate([_pm(bada), _pm(gmix), _pm(gffn)], 1))}


def host_ab(j, win, dlam, subln, gq, gkv, wuq, wukv, wout, lam_init=None):
    abv = np.zeros((128, 8), np.float32)
    abv[:, 0] = gq[:128]
    abv[:, 1] = gq[128:]
    abv[:, 2] = gkv
    abv[:, 3] = subln
    return {f"win{j}": np.ascontiguousarray(win, np.float32), f"abvec{j}": abv,
            f"dlam{j}": np.ascontiguousarray(np.tile(np.asarray(dlam, np.float32).reshape(1, 256), (128, 1))),
            f"wuq{j}": np.ascontiguousarray(wuq, np.float32), f"wukv{j}": np.ascontiguousarray(wukv, np.float32),
            f"woutab{j}": np.ascontiguousarray(wout, np.float32)}


def host_groups(x_b, ctx_b):
    out = np.empty((4, D, T), np.float32)
    for g in range(4):
        out[g, :, :NLAT] = x_b[g * NLAT:(g + 1) * NLAT].T
        out[g, :, NLAT:] = ctx_b[g * NCTX:(g + 1) * NCTX].T
    return out


def host_moe(l, wr, br, w1, b1, w2, b2, n_exp=NE):
    ne = b1.shape[0]
    w1 = w1[:n_exp]
    w2 = w2[:n_exp]
    b1g = b1[:, 0::2].reshape(ne, 8, 128).transpose(2, 0, 1)
    b1l = b1[:, 1::2].reshape(ne, 8, 128).transpose(2, 0, 1)
    b2t = b2.reshape(ne, 8, 128).transpose(2, 0, 1)
    return {f"wr{l}": np.ascontiguousarray(wr.reshape(8, 128, NE).transpose(1, 0, 2).reshape(128, 8 * NE)),
            f"br{l}": np.ascontiguousarray(np.tile(br[None, :], (128, 1))),
            f"w1_{l}": np.ascontiguousarray(np.concatenate([w1[:, :, 0::2], w1[:, :, 1::2]], -1)),
            f"w2_{l}": np.ascontiguousarray(w2),
            f"bexp{l}": np.ascontiguousarray(np.concatenate([b1g, b1l, b2t], -1).reshape(128, ne * 24)).astype(np.float32)}


def host_c(j, l, winc, lb_raw, hg, woutc):
    return {f"winc{j}": np.ascontiguousarray(winc, np.float32),
            "lbraw": np.ascontiguousarray(np.tile(np.asarray(lb_raw, np.float32).reshape(1, 4 * D), (128, 1))),
            f"hgn{j}": np.ascontiguousarray(np.asarray(hg, np.float32).reshape(128, 1)),
            f"woutc{j}": np.ascontiguousarray(woutc, np.float32)}


def _add_c_methods():
    NCHK = NKEY // 64
    NBLK = NCHK // 4

    def c_scratch(self):
        nc = self.nc
        if hasattr(self, "cQ"):
            return
        self.cQ = nc.dram_tensor("cQ", [8, 128, NKEY], BF16).ap()
        self.cSG = nc.dram_tensor("cSG", [8, 128, NKEY], BF16).ap()
        self.cLF = [nc.dram_tensor(f"cLF{d}", [8, NKEY, 128], F32).ap() for d in range(2)]
        self.cK = [nc.dram_tensor(f"cK{d}", [8, NKEY, 128], BF16).ap() for d in range(2)]
        self.cV = nc.dram_tensor("cV", [8, NKEY, 128], BF16).ap()
        self.cO = [nc.dram_tensor(f"cO{d}", [8, 128, NKEY], F32).ap() for d in range(2)]
        self.cp_t = [Tr() for _ in range(4)]
        self.co_t = [Tr(), Tr()]

    def c_lb(self, l):
        P = self.P
        lb_d = self.inp("lbraw", [128, 4 * D])
        raw = P.sbuf("lbraw", [128, 4, D], F32)
        raw_t = Tr()
        P.dma("sp", raw[:].rearrange("p l d -> p (l d)"), lb_d[:, :], [], [raw_t])
        self.LB = P.sbuf("LBrow", [128, D], F32, mid=True)
        self.OML = P.sbuf("OMLrow", [128, D], F32, mid=True)
        self.lb_t = Tr()
        mx = P.sbuf("lbmx", [128, D], F32)
        mx_t = Tr()
        sm = P.sbuf("lbsm", [128, D], F32)
        sm_t = Tr()
        P.tt("dve", mx[:], raw[:, 0, :], raw[:, 1, :], ALU.max, [raw_t], [mx_t])
        P.tt("dve", mx[:], mx[:], raw[:, 2, :], ALU.max, [raw_t, mx_t], [mx_t])
        P.tt("dve", mx[:], mx[:], raw[:, 3, :], ALU.max, [raw_t, mx_t], [mx_t])
        for i in range(4):
            P.tt("dve", raw[:, i, :], raw[:, i, :], mx[:], ALU.subtract, [raw_t, mx_t], [raw_t])
        P.act(raw[:].rearrange("p l d -> p (l d)"), raw[:].rearrange("p l d -> p (l d)"), AF.Exp, [raw_t], [raw_t])
        P.tt("dve", sm[:], raw[:, 0, :], raw[:, 1, :], ALU.add, [raw_t], [sm_t])
        P.tt("dve", sm[:], sm[:], raw[:, 2, :], ALU.add, [raw_t, sm_t], [sm_t])
        P.tt("dve", sm[:], sm[:], raw[:, 3, :], ALU.add, [raw_t, sm_t], [sm_t])
        P.op("dve", lambda e: e.reciprocal(sm[:], sm[:]), [sm_t], [sm_t])
        P.copy("dve", mx[:], raw[:, 1, :], [raw_t], [mx_t])
        for i in range(2, l + 1):
            P.tt("dve", mx[:], mx[:], raw[:, i, :], ALU.add, [raw_t, mx_t], [mx_t])
        P.tt("dve", self.LB[:], mx[:], sm[:], ALU.mult, [mx_t, sm_t], [self.lb_t])
        P.ts("dve", self.OML[:], self.LB[:], -1.0, 1.0, ALU.mult, ALU.add, [self.lb_t], [self.lb_t])

    def c_proj(self, j, g, uT, uT_t):
        P = self.P
        self.c_scratch()
        mk = lambda nm, shp, dt, n: Rot([(P.sbuf(f"{nm}{i}", shp, dt), Tr()) for i in range(n)])
        wq_d = self.inp(f"winc{j}", [D, 5 * D])
        wv = wq_d.rearrange("(k p) f -> p k f", p=128)
        wbuf = mk("cw", [128, 8, 512], BF16, 2)
        ps = Rot([(P.psum(), Tr(ps=True)) for _ in range(4)])
        fm = mk("cfm", [128, 512], BF16, 3)
        sgm = mk("csg", [128, 512], F32, 2)
        fz = mk("cfz", [128, 512], F32, 2)
        lf = mk("clf", [128, 512], F32, 2)
        kk = mk("ckk", [128, 512], BF16, 2)
        vv = mk("cvv", [128, 512], BF16, 2)
        pt = self.cp_t[g]
        for piece in range(10):
            blk = piece // 2
            wt, wt_t = wbuf.next()
            P.dma("pool", wt[:], wv[:, :, piece * 512:(piece + 1) * 512], [], [wt_t])
            h0 = (piece % 2) * 4
            for ti, (s, w) in enumerate(TILES):
                kc0 = self.key_cols(g, ti)
                if blk in (0, 4):
                    for hh in range(4):
                        p_, p_t = ps.next()
                        for k in range(8):
                            P.mm(p_[:, :w], wt[:, k, hh * 128:(hh + 1) * 128], uT[:, k, s:s + w], k == 0, k == 7,
                                 [wt_t, uT_t[ti]], [p_t])
                        o_, o_t = fm.next()
                        if blk == 0:
                            P.copy("act", o_[:, :w], p_[:, :w], [p_t], [o_t])
                            P.dma("sp", self.cQ[h0 + hh, :, kc0:kc0 + w], o_[:, :w], [o_t], [pt])
                        else:
                            P.act(o_[:, :w], p_[:, :w], AF.Silu, [p_t], [o_t])
                            P.dma("sp", self.cSG[h0 + hh, :, kc0:kc0 + w], o_[:, :w], [o_t], [pt])
                else:
                    for b0 in range(0, w, 128):
                        bw = min(128, w - b0)
                        p_, p_t = ps.next()
                        for k in range(8):
                            P.mm(p_[:bw, :], uT[:, k, s + b0:s + b0 + bw], wt[:, k, :], k == 0, k == 7,
                                 [uT_t[ti], wt_t], [p_t])
                        if blk == 3:
                            v_, v_t = vv.next()
                            P.copy("act", v_[:bw, :], p_[:bw, :], [p_t], [v_t])
                            P.dma("sp", self.cV[h0:h0 + 4, kc0 + b0:kc0 + b0 + bw, :].rearrange("h k d -> k h d"),
                                  v_[:bw, :].rearrange("k (h d) -> k h d", h=4), [v_t], [pt])
                        else:
                            dr = blk - 1
                            c0 = h0 * 128
                            sg_, sg_t = sgm.next()
                            f_, f_t = fz.next()
                            l_, l_t = lf.next()
                            k_, k_t = kk.next()
                            P.act(sg_[:bw, :], p_[:bw, :], AF.Sigmoid, [p_t], [sg_t])
                            P.tt("dve", f_[:bw, :], sg_[:bw, :], self.OML[:bw, c0:c0 + 512], ALU.mult, [sg_t, self.lb_t], [f_t])
                            P.tt("dve", f_[:bw, :], f_[:bw, :], self.LB[:bw, c0:c0 + 512], ALU.add, [f_t, self.lb_t], [f_t])
                            P.act(l_[:bw, :], f_[:bw, :], AF.Ln, [f_t], [l_t])
                            P.ts("dve", k_[:bw, :], f_[:bw, :], -1.0, 1.0, ALU.mult, ALU.add, [f_t], [k_t])
                            P.dma("sp", self.cLF[dr][h0:h0 + 4, kc0 + b0:kc0 + b0 + bw, :].rearrange("h k d -> k h d"),
                                  l_[:bw, :].rearrange("k (h d) -> k h d", h=4), [l_t], [pt])
                            P.dma("sp", self.cK[dr][h0:h0 + 4, kc0 + b0:kc0 + b0 + bw, :].rearrange("h k d -> k h d"),
                                  k_[:bw, :].rearrange("k (h d) -> k h d", h=4), [k_t], [pt])

    def c_scan(self):
        P = self.P
        mk = lambda nm, shp, dt, n: Rot([(P.sbuf(f"{nm}{i}", shp, dt), Tr()) for i in range(n)])
        U = []
        Um = []
        for d_ in range(2):
            u = P.sbuf(f"U{d_}", [64, 64], F32)
            u_t = Tr()
            P.memset("dve", u[:], 1.0, [u_t])
            cm, coef = (-1, 1) if d_ == 0 else (1, -1)
            P.op("pool", lambda e, u=u, cm=cm, coef=coef: e.affine_select(u[:], u[:], [[coef, 64]], ALU.is_ge, 0.0, base=0,
                                                                          channel_multiplier=cm), [u_t], [u_t])
            U.append((u, u_t))
        NS = 4
        S = [[(P.sbuf(f"S{sl}", [128, 128], F32), Tr()), (P.sbuf(f"Sb{sl}", [128, 128], BF16), Tr())] for sl in range(NS)]
        lfb = [mk(f"slf{sl}", [64, 4, 128], F32, 2) for sl in range(NS)]
        kb_ = [mk(f"sk{sl}", [64, 4, 128], BF16, 2) for sl in range(NS)]
        vb_ = [mk(f"sv{sl}", [64, 4, 128], BF16, 2) for sl in range(NS)]
        qb_ = [mk(f"sq{sl}", [128, 256], BF16, 2) for sl in range(NS)]
        ob_ = [mk(f"so{sl}", [128, 256], F32, 2) for sl in range(NS)]
        eq = mk("seq", [128, 64], F32, 6)
        ek = mk("sek", [64, 128], F32, 6)
        qt_ = mk("sqt", [128, 64], BF16, 6)
        kt_ = mk("skt", [64, 128], BF16, 6)
        ktT = mk("sktT", [128, 64], BF16, 6)
        at_ = mk("sat", [64, 64], BF16, 6)
        tS = mk("stS", [128, 128], F32, 4)
        banks = [(P.psum(), Tr(ps=True)) for _ in range(8)]
        pc = Rot(banks[0:2])
        pa = Rot(banks[2:4])
        po = Rot(banks[4:6])
        pd = Rot(banks[6:8])
        allp = self.cp_t
        order = [list(range(NBLK)), [0] + list(range(NBLK - 1, 0, -1))]
        for hp in range(4):
            for sl in range(NS):
                (s_, s_t), (sb, sb_t) = S[sl]
                P.memset("dve", s_[:], 0.0, [s_t])
                P.memset("pool", sb[:], 0.0, [sb_t])
            for bi in range(NBLK):
                cur = []
                for sl in range(NS):
                    h = 2 * hp + sl // 2
                    d_ = sl % 2
                    b = order[d_][bi]
                    c0 = b * 256
                    l_, l_t = lfb[sl].next()
                    k_, k_t = kb_[sl].next()
                    v_, v_t = vb_[sl].next()
                    q_, q_t = qb_[sl].next()
                    o_, o_t = ob_[sl].next()
                    P.dma("sp", l_[:], self.cLF[d_][h, c0:c0 + 256, :].rearrange("(c s) d -> s c d", s=64), allp, [l_t])
                    P.dma("sp", k_[:], self.cK[d_][h, c0:c0 + 256, :].rearrange("(c s) d -> s c d", s=64), allp, [k_t])
                    P.dma("sp", v_[:], self.cV[h, c0:c0 + 256, :].rearrange("(c s) d -> s c d", s=64), allp, [v_t])
                    P.dma("sp", q_[:], self.cQ[h, :, c0:c0 + 256], allp, [q_t])
                    cur.append((h, d_, c0, l_, l_t, k_, k_t, v_, v_t, q_, q_t, o_, o_t))
                for ci in range(4):
                    for sl in range(NS):
                        h, d_, c0, l_, l_t, k_, k_t, v_, v_t, q_, q_t, o_, o_t = cur[sl]
                        c = ci if d_ == 0 else 3 - ci
                        u, u_t = U[d_]
                        (s_, s_t), (sb, sb_t) = S[sl]
                        p1, p1_t = pc.next()
                        P.mm(p1[:, 0:64], l_[:, c, :], u[:, :], True, True, [l_t, u_t], [p1_t])
                        P.mm(p1[:64, 128:256], u[:, :], l_[:, c, :], True, True, [u_t, l_t], [p1_t])
                        e1, e1_t = eq.next()
                        e2, e2_t = ek.next()
                        P.act(e1[:, :], p1[:, 0:64], AF.Exp, [p1_t], [e1_t])
                        P.act(e2[:, :], p1[:64, 128:256], AF.Exp, [p1_t], [e2_t], scale=-1.0)
                        qt, qt_t = qt_.next()
                        kt, kt_t = kt_.next()
                        P.tt("dve", qt[:, :], q_[:, c * 64:(c + 1) * 64], e1[:, :], ALU.mult, [q_t, e1_t], [qt_t])
                        P.tt("dve", kt[:, :], k_[:, c, :], e2[:, :], ALU.mult, [k_t, e2_t], [kt_t])
                        p2, p2_t = pa.next()
                        p2b = p2[:].bitcast(BF16)
                        P.tp(p2b[:, 0:64], kt[:, :], self.identb[:64, :64], [kt_t, self.identb_t], [p2_t])
                        kT, kT_t = ktT.next()
                        P.copy("act", kT[:, :], p2b[:, 0:64], [p2_t], [kT_t])
                        P.mm(p2[:64, 256:320], kT[:, :], qt[:, :], True, True, [kT_t, qt_t], [p2_t])
                        at, at_t = at_.next()
                        P.tt("dve", at[:, :], p2[:64, 256:320], u[:, :], ALU.mult, [p2_t, u_t], [at_t])
                        p3, p3_t = po.next()
                        P.mm(p3[:, 0:64], v_[:, c, :], at[:, :], True, False, [v_t, at_t], [p3_t])
                        P.mm(p3[:, 0:64], sb[:, :], qt[:, :], False, True, [sb_t, qt_t], [p3_t])
                        P.copy("act", o_[:, c * 64:(c + 1) * 64], p3[:, 0:64], [p3_t], [o_t])
                        p4, p4_t = pd.next()
                        P.mm(p4[:, 0:128], kt[:, :], v_[:, c, :], True, True, [kt_t, v_t], [p4_t])
                        ts_, ts_t = tS.next()
                        ecol = e1[:, 63:64] if d_ == 0 else e1[:, 0:1]
                        P.tt("dve", ts_[:, :], s_[:, :], p4[:, 0:128], ALU.add, [s_t, p4_t], [ts_t])
                        P.ts("dve", s_[:, :], ts_[:, :], ecol, None, ALU.mult, None, [ts_t, e1_t], [s_t])
                        P.copy("act", sb[:, :], s_[:, :], [s_t], [sb_t])
                for sl in range(NS):
                    h, d_, c0, l_, l_t, k_, k_t, v_, v_t, q_, q_t, o_, o_t = cur[sl]
                    P.dma("sp", self.cO[d_][h, :, c0:c0 + 256], o_[:, :], [o_t], [self.co_t[d_]])

    def c_readout(self, j, g, oT, oT_t):
        P = self.P
        mk = lambda nm, shp, dt, n: Rot([(P.sbuf(f"{nm}{i}", shp, dt), Tr()) for i in range(n)])
        hg_d = self.inp(f"hgn{j}", [128, 1])
        hg = P.sbuf("hgn", [128, 1], F32)
        hg_t = Tr()
        P.dma("sp", hg[:], hg_d[:, :], [], [hg_t])
        of = mk("rof", [128, 512], F32, 2)
        ob = mk("rob", [128, 512], F32, 2)
        sg = mk("rsg", [128, 512], BF16, 2)
        sq = mk("rsq", [128, 512], BF16, 2)
        rs = mk("rrs", [128, 512], F32, 2)
        ps = Rot([(P.psum(), Tr(ps=True)) for _ in range(2)])
        for h in range(8):
            for ti, (s, w) in enumerate(TILES):
                kc0 = self.key_cols(g, ti)
                a, a_t = of.next()
                b, b_t = ob.next()
                c, c_t = sg.next()
                P.dma("sp", a[:, :w], self.cO[0][h, :, kc0:kc0 + w], [self.co_t[0]], [a_t])
                P.dma("sp", b[:, :w], self.cO[1][h, :, kc0:kc0 + w], [self.co_t[1]], [b_t])
                P.dma("sp", c[:, :w], self.cSG[h, :, kc0:kc0 + w], self.cp_t, [c_t])
                P.tt("dve", a[:, :w], a[:, :w], b[:, :w], ALU.add, [a_t, b_t], [a_t])
                q, q_t = sq.next()
                P.act(q[:, :w], a[:, :w], AF.Square, [a_t], [q_t])
                p, p_t = ps.next()
                P.mm(p[:, :w], self.onesb[:, :], q[:, :w], True, True, [self.onesb_t, q_t], [p_t])
                r, r_t = rs.next()
                P.act(r[:, :w], p[:, :w], AF.Sqrt, [p_t], [r_t], bias=EPS, scale=1.0 / 128)
                P.op("dve", lambda e, x=r[:, :w]: e.reciprocal(x, x), [r_t], [r_t])
                P.stt("dve", a[:, :w], a[:, :w], hg[:, 0:1], r[:, :w], ALU.mult, ALU.mult, [a_t, hg_t, r_t], [a_t])
                P.tt("dve", oT[:, h, s:s + w], a[:, :w], c[:, :w], ALU.mult, [a_t, c_t], [oT_t[ti]])

    def c_layer(self, l, groups=(0, 1, 2, 3), out_groups=(0, 1, 2, 3), do_moe=True, n_exp=NE):
        P = self.P
        j = l // 2
        self.h_scratch()
        self.c_scratch()
        self.mods(l)
        P.flush()
        m0 = P.mark()
        self.c_lb(l)
        P.flush()
        for g in groups:
            uT = P.sbuf("uT", [128, 8, T], BF16)
            uT_t = [Tr() for _ in TILES]
            src, src_t = self.h_src(l, g)
            self.norm_stream(src, src_t, uT, uT_t, 0)
            self.c_proj(j, g, uT, uT_t)
            P.flush()
        P.release(m0)
        self.c_scan()
        P.flush()
        for g in out_groups:
            m1 = P.mark()
            oT = P.sbuf("oT", [128, 8, T], BF16, mid=True)
            oT_t = [Tr() for _ in TILES]
            self.c_readout(j, g, oT, oT_t)
            P.flush()
            hT = P.sbuf("hT", [128, 8, T], F32, mid=True)
            hT_t = [Tr() for _ in TILES]
            src, src_t = self.h_src(l, g)
            self.load_h(src, src_t, hT, hT_t)
            self.out_proj(f"woutc{j}", hT, hT_t, oT, oT_t)
            P.flush()
            if do_moe:
                vT_t = [Tr() for _ in TILES]
                self.moe(l, hT, hT_t, oT, vT_t, n_exp=n_exp, skip_ctx=(l == DEPTH - 1))
            self.store_h(self.H[g], [self.H_t[g]], hT, hT_t)
            P.flush()
            P.release(m1)

    for f in (c_scratch, c_lb, c_proj, c_scan, c_readout, c_layer):
        setattr(LB, f.__name__, f)


_add_c_methods()


def _final_norm(self, g, out_d):
    P = self.P
    fg_d = self.inp("fing", [128, 8])
    fg = P.sbuf("fing", [128, 8], F32)
    fg_t = Tr()
    P.dma("sp", fg[:], fg_d[:, :], [], [fg_t])
    hv = self.H[g].rearrange("(c p) t -> p c t", p=128)
    ov = out_d[g].rearrange("(c p) t -> p c t", p=128)
    hb = [(P.sbuf(f"fh{i}", [128, 8, 512], F32), Tr()) for i in range(2)]
    sq = [(P.sbuf(f"fsq{i}", [128, 8, 512], BF16), Tr()) for i in range(1)]
    rs = [(P.sbuf(f"frs{i}", [128, 512], F32), Tr()) for i in range(2)]
    ps = [(P.psum(), Tr(ps=True)) for _ in range(2)]
    for ti in range(4):
        s, w = TILES[ti]
        h_, h_t = hb[ti % 2]
        q_, q_t = sq[0]
        r_, r_t = rs[ti % 2]
        p_, p_t = ps[ti % 2]
        P.dma("sp", h_[:, :, :w], hv[:, :, s:s + w], [self.H_t[g]], [h_t])
        for c in range(8):
            P.act(q_[:, c, :w], h_[:, c, :w], AF.Square, [h_t], [q_t])
        for c in range(8):
            P.mm(p_[:, :w], self.onesb[:, :], q_[:, c, :w], c == 0, c == 7, [self.onesb_t, q_t], [p_t])
        P.act(r_[:, :w], p_[:, :w], AF.Sqrt, [p_t], [r_t], bias=EPS, scale=1.0 / D)
        P.op("dve", lambda e, a=r_[:, :w]: e.reciprocal(a, a), [r_t], [r_t])
        for c in range(8):
            P.stt("dve", h_[:, c, :w], h_[:, c, :w], fg[:, c:c + 1], r_[:, :w], ALU.mult, ALU.mult, [h_t, fg_t, r_t], [h_t])
        P.dma("sp", ov[:, :, s:s + w], h_[:, :, :w], [h_t], [])


LB.final_norm = _final_norm


def build_program(n_exp=NE, layers=(0, 1, 2, 3)):
    B = LB()
    P = B.P
    B.consts()
    P.flush()
    B.h_scratch()
    if 0 not in layers:
        for g in range(4):
            P.dma("sp", B.H[g], B.xT[g], [], [B.H_t[g]])
        P.flush()
    for l in layers:
        if l % 2 == 0:
            B.ab_layer(l, n_exp=n_exp)
        else:
            B.c_layer(l, n_exp=n_exp)
    out_d = B.out("outT", [4, D, NLAT])
    for g in range(4):
        B.final_norm(g, out_d)
        P.flush()
    P.close()
    return B


def host_inputs(inp, b, n_exp=NE, layers=(0, 1, 2, 3)):
    f = lambda a: np.asarray(a, np.float32)
    m = host_common(f(inp["c"])[b], f(inp["c_ctx"]))
    m["xT"] = host_groups(f(inp["x"])[b], f(inp["ctx"])[b])
    m["fing"] = _pm(f(inp["final_g"]))
    for l in layers:
        j = l // 2
        m.update(host_layer_vec(l, f(inp["w_ada"])[l], f(inp["b_ada"])[l], f(inp["norm_mix_g"])[l], f(inp["norm_ffn_g"])[l]))
        if l % 2 == 0:
            m.update(host_ab(j, f(inp["w_in_ab"])[j], f(inp["diff_lambda"])[j], f(inp["diff_subln_g"])[j],
                             f(inp["mla_q_norm_g"])[j], f(inp["mla_kv_norm_g"])[j], f(inp["w_uq"])[j], f(inp["w_ukv"])[j],
                             f(inp["w_out_ab"])[j]))
        else:
            m.update(host_c(j, l, f(inp["w_in_c"])[j], f(inp["lb_raw"]), f(inp["hgrn_norm_g"])[j], f(inp["w_out_c"])[j]))
        m.update(host_moe(l, f(inp["w_router"])[l], f(inp["b_router"])[l], inp["w_exp1"][l], f(inp["b_exp1"])[l],
                          inp["w_exp2"][l], f(inp["b_exp2"])[l], n_exp))
    return m


def kernel(**inputs):
    B = build_program()
    in_maps = []
    for b in range(2):
        m = host_inputs(inputs, b)
        in_maps.append({k: m[k] for k in B.ins})
    res = run_bass_kernel_spmd(B.nc, in_maps, core_ids=[0, 1])
    out = np.empty((2, SEQ, D), np.float32)
    for b in range(2):
        oT = res.results[b]["outT"]
        for g in range(4):
            out[b, g * NLAT:(g + 1) * NLAT, :] = oT[g].T
    return out
```
